# Optimizing a Trainium2 kernel written in Bass

```python
import math
import jax, jax.numpy as jnp
from jax import lax
import numpy as np

D_MODEL = 1024
BATCH = 8
SEQ = 4096
DEPTH = 1

D_MIX = D_MODEL
D_RWKV = D_MIX // 2
D_SSM = D_MIX - D_RWKV
HEAD_DIM = 64
N_RWKV_HEADS = D_RWKV // HEAD_DIM
DECAY_LORA = 64
AAA_LORA = 64
GATE_LORA = 128
N_SHIFT = 3 * D_RWKV + DECAY_LORA + AAA_LORA + GATE_LORA
D_IN = N_SHIFT + D_SSM
SSM_GROUP = 16
N_SSM_GROUPS = D_SSM // SSM_GROUP
SSM_STATE = 64
DT_MIN = 1e-3
DT_MAX = 1e-1
N_EXPERTS = 32
TOP_K = 4
D_FF = D_MODEL
SWIGLU_ALPHA = 1.702
SWIGLU_LIMIT = 7.0
NORM_EPS = 1e-5
LN_X_EPS = 64e-5

kernel_name = "hymba_rwkv7_s5_moe_block"


def rmsnorm(x, g):
    x32 = x.astype(jnp.float32)
    y = x32 * lax.rsqrt(jnp.mean(x32 * x32, axis=-1, keepdims=True) + NORM_EPS)
    return (y * g.astype(jnp.float32)).astype(x.dtype)


def token_shift(p):
    return jnp.pad(p[:, :-1], ((0, 0), (1, 0), (0, 0)))


def rwkv7_recurrence(r, w, k, v, kk, b):
    bsz, _, h, n = r.shape
    xs = tuple(jnp.moveaxis(t.astype(jnp.float32), 1, 0) for t in (r, w, k, v, kk, b))

    def step(S, inp):
        r_t, w_t, k_t, v_t, kk_t, b_t = inp
        sa = jnp.einsum('bhvk,bhk->bhv', S, -kk_t)
        S = S * w_t[:, :, None, :] + sa[..., None] * b_t[:, :, None, :] + v_t[..., None] * k_t[:, :, None, :]
        y = jnp.einsum('bhvk,bhk->bhv', S, r_t)
        return S, y

    S0 = jnp.zeros((bsz, h, n, n), jnp.float32)
    _, y = lax.scan(step, S0, xs)
    return jnp.moveaxis(y, 0, 1)


def rwkv7_mix(p, w0, w_up, a0, a_up, g_up, k_k, k_a, r_k, ln_w, ln_b):
    bsz, t_len, _ = p.shape
    H, N = N_RWKV_HEADS, HEAD_DIM
    c1 = 3 * D_RWKV
    r, k, v, w_lr, a_lr, g_lr = jnp.split(
        p, [D_RWKV, 2 * D_RWKV, c1, c1 + DECAY_LORA, c1 + DECAY_LORA + AAA_LORA], axis=-1)
    log_w = -jax.nn.softplus(-(w0 + jnp.tanh(w_lr) @ w_up)) - 0.5
    decay = jnp.exp(-jnp.exp(log_w.astype(jnp.float32)))
    a = jax.nn.sigmoid(a0 + a_lr @ a_up)
    g = jax.nn.sigmoid(g_lr) @ g_up

    def heads(t):
        return t.reshape(bsz, t_len, H, N)

    kk = heads(k * k_k).astype(jnp.float32)
    kk = kk / jnp.maximum(jnp.sqrt(jnp.sum(kk * kk, axis=-1, keepdims=True)), 1e-12)
    k = k * (1.0 + (a - 1.0) * k_a)
    y = rwkv7_recurrence(heads(r), heads(decay), heads(k), heads(v), kk, kk * heads(a))
    mu = jnp.mean(y, axis=-1, keepdims=True)
    var = jnp.mean(jnp.square(y - mu), axis=-1, keepdims=True)
    y = ((y - mu) * lax.rsqrt(var + LN_X_EPS)).reshape(bsz, t_len, D_RWKV) * ln_w + ln_b
    bonus = jnp.sum(heads(r) * heads(k) * r_k, axis=-1, keepdims=True) * heads(v)
    y = (y + bonus.reshape(bsz, t_len, D_RWKV)) * g
    return y.astype(p.dtype)


def s5_mix(u, lambda_re, lambda_im, log_step, b_re, b_im, c_re, c_im, d_skip, w_glu, b_glu):
    bsz, t_len, _ = u.shape
    G, P, M = N_SSM_GROUPS, SSM_STATE, SSM_GROUP
    f32 = jnp.float32
    u32 = u.astype(f32)
    ug = u32.reshape(bsz, t_len, G, M)
    lam_re = jnp.minimum(lambda_re.astype(f32), -1e-4)
    lam_im = lambda_im.astype(f32)
    dt = jnp.exp(log_step.astype(f32))[:, None]
    mag = jnp.exp(lam_re * dt)
    lb_re = mag * jnp.cos(lam_im * dt)
    lb_im = mag * jnp.sin(lam_im * dt)
    den = lam_re * lam_re + lam_im * lam_im
    num_re = lb_re - 1.0
    z_re = (num_re * lam_re + lb_im * lam_im) / den
    z_im = (lb_im * lam_re - num_re * lam_im) / den
    b_re32, b_im32 = b_re.astype(f32), b_im.astype(f32)
    bb_re = z_re[..., None] * b_re32 - z_im[..., None] * b_im32
    bb_im = z_re[..., None] * b_im32 + z_im[..., None] * b_re32
    bu_re = jnp.einsum('btgm,gpm->tbgp', ug, bb_re)
    bu_im = jnp.einsum('btgm,gpm->tbgp', ug, bb_im)
    a_re = jnp.broadcast_to(lb_re[None, None], (t_len, 1, G, P))
    a_im = jnp.broadcast_to(lb_im[None, None], (t_len, 1, G, P))

    def combine(e1, e2):
        a1r, a1i, b1r, b1i = e1
        a2r, a2i, b2r, b2i = e2
        return (a1r * a2r - a1i * a2i,
                a1r * a2i + a1i * a2r,
                a2r * b1r - a2i * b1i + b2r,
                a2r * b1i + a2i * b1r + b2i)

    _, _, s_re, s_im = lax.associative_scan(combine, (a_re, a_im, bu_re, bu_im), axis=0)
    y = (jnp.einsum('tbgp,gmp->btgm', s_re, c_re.astype(f32))
         - jnp.einsum('tbgp,gmp->btgm', s_im, c_im.astype(f32)))
    y = y.reshape(bsz, t_len, D_SSM) + d_skip.astype(f32) * u32
    y = jax.nn.gelu(y)
    y = y * jax.nn.sigmoid(y @ w_glu.astype(f32) + b_glu.astype(f32))
    return y


def moe_ffn(h, router_w, router_b, w1, b1, w2, b2):
    bsz, t_len, d = h.shape
    ht = h.reshape(-1, d)
    logits = (ht @ router_w + router_b).astype(jnp.float32)
    top_val, top_idx = lax.top_k(logits, TOP_K)
    gates = jax.nn.softmax(top_val, axis=-1)
    comb = jnp.einsum('nk,nke->ne', gates, jax.nn.one_hot(top_idx, N_EXPERTS, dtype=jnp.float32))
    out = jnp.zeros((ht.shape[0], d), jnp.float32)
    for e in range(N_EXPERTS):
        hu = ht @ w1[e] + b1[e]
        x_glu = jnp.minimum(hu[:, 0::2], SWIGLU_LIMIT)
        x_lin = jnp.clip(hu[:, 1::2], -SWIGLU_LIMIT, SWIGLU_LIMIT)
        act = x_glu * jax.nn.sigmoid(SWIGLU_ALPHA * x_glu) * (x_lin + 1.0)
        out = out + comb[:, e:e + 1] * (act @ w2[e] + b2[e])
    return out.reshape(bsz, t_len, d).astype(h.dtype)


def setup_inputs(seed: int = 0) -> dict:
    key = jax.random.key(seed)
    ks = jax.random.split(key, 40)
    L = DEPTH
    f32 = jnp.float32

    def nrm(i, shape, scale):
        return scale * jax.random.normal(ks[i], shape, f32)

    def gain(i, shape):
        return 1.0 + nrm(i, shape, 0.02)

    x = nrm(0, (BATCH, SEQ, D_MODEL), 1.0)
    norm1_g = gain(1, (L, D_MODEL))
    w_in = nrm(2, (L, D_MODEL, D_IN), D_MODEL ** -0.5)
    mu_shift = jax.random.uniform(ks[3], (L, N_SHIFT), f32)
    ratio = jnp.arange(L, dtype=f32)[:, None] / max(DEPTH - 1, 1)
    frac = jnp.arange(D_RWKV, dtype=f32)[None, :] / (D_RWKV - 1)
    w0 = -7.0 + 5.0 * frac ** (0.85 + jnp.sqrt(ratio)) + 0.5 + nrm(4, (L, D_RWKV), 0.02)
    w_up = nrm(5, (L, DECAY_LORA, D_RWKV), 0.1 * DECAY_LORA ** -0.5)
    a0 = nrm(6, (L, D_RWKV), 0.1)
    a_up = nrm(7, (L, AAA_LORA, D_RWKV), 0.1 * AAA_LORA ** -0.5)
    g_up = nrm(8, (L, GATE_LORA, D_RWKV), GATE_LORA ** -0.5)
    k_k = 0.85 + nrm(9, (L, D_RWKV), 0.02)
    k_a = 1.0 + nrm(10, (L, D_RWKV), 0.02)
    r_k = -0.04 + nrm(11, (L, N_RWKV_HEADS, HEAD_DIM), 0.02)
    ln_x_w = gain(12, (L, D_RWKV))
    ln_x_b = nrm(13, (L, D_RWKV), 0.01)
    lambda_re = -0.5 + nrm(14, (L, N_SSM_GROUPS, SSM_STATE), 0.01)
    lambda_im = (jnp.pi * jnp.arange(SSM_STATE, dtype=f32))[None, None, :] + nrm(15, (L, N_SSM_GROUPS, SSM_STATE), 0.01)
    log_step = jax.random.uniform(ks[16], (L, N_SSM_GROUPS), f32, math.log(DT_MIN), math.log(DT_MAX))
    b_re = nrm(17, (L, N_SSM_GROUPS, SSM_STATE, SSM_GROUP), (2 * SSM_GROUP) ** -0.5)
    b_im = nrm(18, (L, N_SSM_GROUPS, SSM_STATE, SSM_GROUP), (2 * SSM_GROUP) ** -0.5)
    c_re = nrm(19, (L, N_SSM_GROUPS, SSM_GROUP, SSM_STATE), (2 * SSM_STATE) ** -0.5)
    c_im = nrm(20, (L, N_SSM_GROUPS, SSM_GROUP, SSM_STATE), (2 * SSM_STATE) ** -0.5)
    d_skip = nrm(21, (L, D_SSM), 1.0)
    w_glu = nrm(22, (L, D_SSM, D_SSM), D_SSM ** -0.5)
    b_glu = nrm(23, (L, D_SSM), 0.01)
    beta_ssm = gain(24, (L, D_SSM))
    w_out = nrm(25, (L, D_MIX, D_MODEL), D_MIX ** -0.5)
    norm2_g = gain(26, (L, D_MODEL))
    router_w = nrm(27, (L, D_MODEL, N_EXPERTS), D_MODEL ** -0.5)
    router_b = nrm(28, (L, N_EXPERTS), 0.01)
    w1 = nrm(29, (L, N_EXPERTS, D_MODEL, 2 * D_FF), D_MODEL ** -0.5)
    b1 = nrm(30, (L, N_EXPERTS, 2 * D_FF), 0.01)
    w2 = nrm(31, (L, N_EXPERTS, D_FF, D_MODEL), D_FF ** -0.5)
    b2 = nrm(32, (L, N_EXPERTS, D_MODEL), 0.01)
    final_g = gain(33, (D_MODEL,))
    return {"x": x, "norm1_g": norm1_g, "w_in": w_in, "mu_shift": mu_shift,
            "w0": w0, "w_up": w_up, "a0": a0, "a_up": a_up, "g_up": g_up,
            "k_k": k_k, "k_a": k_a, "r_k": r_k, "ln_x_w": ln_x_w, "ln_x_b": ln_x_b,
            "lambda_re": lambda_re, "lambda_im": lambda_im, "log_step": log_step,
            "b_re": b_re, "b_im": b_im, "c_re": c_re, "c_im": c_im, "d_skip": d_skip,
            "w_glu": w_glu, "b_glu": b_glu, "beta_ssm": beta_ssm, "w_out": w_out,
            "norm2_g": norm2_g, "router_w": router_w, "router_b": router_b,
            "w1": w1, "b1": b1, "w2": w2, "b2": b2, "final_g": final_g}


def reference(x, norm1_g, w_in, mu_shift, w0, w_up, a0, a_up, g_up, k_k, k_a, r_k,
              ln_x_w, ln_x_b, lambda_re, lambda_im, log_step, b_re, b_im, c_re, c_im,
              d_skip, w_glu, b_glu, beta_ssm, w_out, norm2_g, router_w, router_b,
              w1, b1, w2, b2, final_g):
    for l in range(DEPTH):
        h = rmsnorm(x, norm1_g[l])
        proj = h @ w_in[l]
        ps, u = proj[..., :N_SHIFT], proj[..., N_SHIFT:]
        ps = ps + mu_shift[l] * (token_shift(ps) - ps)
        y_rwkv = rwkv7_mix(ps, w0[l], w_up[l], a0[l], a_up[l], g_up[l], k_k[l], k_a[l], r_k[l],
                           ln_x_w[l], ln_x_b[l])
        y_ssm = rmsnorm(s5_mix(u, lambda_re[l], lambda_im[l], log_step[l], b_re[l], b_im[l],
                               c_re[l], c_im[l], d_skip[l], w_glu[l], b_glu[l]), beta_ssm[l])
        mixed = jnp.concatenate([y_rwkv.astype(x.dtype), y_ssm.astype(x.dtype)], axis=-1)
        x = x + mixed @ w_out[l]
        x = x + moe_ffn(rmsnorm(x, norm2_g[l]), router_w[l], router_b[l], w1[l], b1[l], w2[l], b2[l])
    return rmsnorm(x, final_g)
```

```python
import contextlib
import numpy as np
import concourse.bass as bass
import concourse.mybir as mybir

F32 = mybir.dt.float32
BF16 = mybir.dt.bfloat16
I32 = mybir.dt.int32
AF = mybir.ActivationFunctionType
ALU = mybir.AluOpType
AX = mybir.AxisListType

EPOCH = 30000
SAME_ENG_SYNC = True


class Unit:
    __slots__ = ("name", "w", "r")

    def __init__(self, name):
        self.name = name
        self.w = None
        self.r = {}


class V:
    __slots__ = ("ap", "u")

    def __init__(self, ap, u):
        self.ap = ap
        self.u = u

    def __getitem__(self, k):
        return V(self.ap[k], self.u)

    def re(self, pat, **kw):
        return V(self.ap.rearrange(pat, **kw), self.u)

    def bc(self, shape):
        return V(self.ap.to_broadcast(shape), self.u)

    def bitcast(self, dt):
        return V(self.ap.bitcast(dt), self.u)


def _ap(x):
    return x.ap if isinstance(x, V) else x


class Sched:
    def __init__(self, nc, stack):
        self.nc = nc
        self.stack = stack
        self.engs = {"pe": nc.tensor, "act": nc.scalar, "dve": nc.vector, "pool": nc.gpsimd, "sp": nc.sync}
        self.ops = {e: [] for e in self.engs}
        self.count = {e: 0 for e in self.engs}
        self.seen = {e: {} for e in self.engs}
        self.dmacount = {}
        self.semnames = []
        self.ntile = 0

    def tile(self, shape, dt, name=None):
        self.ntile += 1
        name = name or f"t{self.ntile}"
        t = self.stack.enter_context(self.nc.sbuf_tensor(name, list(shape), dt))
        return V(t.ap() if hasattr(t, "ap") and callable(t.ap) else t[:], Unit(name))

    def psum(self, shape, dt, name=None):
        self.ntile += 1
        name = name or f"p{self.ntile}"
        t = self.stack.enter_context(self.nc.psum_tensor(name, list(shape), dt))
        return V(t.ap() if hasattr(t, "ap") and callable(t.ap) else t[:], Unit(name))

    def op(self, eng, fn, reads=(), writes=(), ring=None):
        waits = {}
        seen = self.seen[eng]

        def need(tok):
            if tok is None:
                return
            sem, val, teng, is_dma = tok
            if teng == eng and not is_dma and (not SAME_ENG_SYNC or eng == 'pe'):
                return
            if seen.get(sem, 0) >= val:
                return
            if waits.get(sem, 0) < val:
                waits[sem] = val

        for x in reads:
            if isinstance(x, V) and x.u is not None:
                need(x.u.w)
        for x in writes:
            if isinstance(x, V) and x.u is not None:
                need(x.u.w)
                for t in x.u.r.values():
                    need(t)
        for s, v in waits.items():
            seen[s] = v
        if ring is not None:
            sem = "d_" + ring
            self.dmacount[sem] = self.dmacount.get(sem, 0) + 16
            tok = (sem, self.dmacount[sem], eng, True)
        else:
            c = self.count[eng]
            self.count[eng] = c + 1
            sem = f"e_{eng}_{c // EPOCH}"
            tok = (sem, c % EPOCH + 1, eng, False)
        if sem not in self.semnames:
            self.semnames.append(sem)
        for s in waits:
            assert s in self.semnames
        self.ops[eng].append((fn, list(waits.items()), tok))
        for x in reads:
            if isinstance(x, V) and x.u is not None:
                old = x.u.r.get(sem)
                if old is None or old[1] < tok[1]:
                    x.u.r[sem] = tok
        for x in writes:
            if isinstance(x, V) and x.u is not None:
                x.u.w = tok
                x.u.r = {}
        return tok

    def emit(self, final_waits_eng="sp"):
        nc = self.nc
        sems = {}
        for n in self.semnames:
            sems[n] = self.stack.enter_context(nc.semaphore(n))
        finals = []
        for e in self.engs:
            c = self.count[e]
            if c:
                finals.append((f"e_{e}_{(c - 1) // EPOCH}", (c - 1) % EPOCH + 1))
        for s, v in self.dmacount.items():
            finals.append((s, v))
        block = self.stack.enter_context(nc.Block())
        ops = self.ops

        def run(eng_handle, name):
            for fn, waits, tok in ops[name]:
                for s, v in waits:
                    eng_handle.wait_ge(sems[s], v)
                if fn is None:
                    continue
                ins = fn(eng_handle)
                ins.then_inc(sems[tok[0]], 16 if tok[3] else 1)
            if name == final_waits_eng:
                for s, v in finals:
                    eng_handle.wait_ge(sems[s], v)

        @block.sync
        def _(e):
            run(e, "sp")

        @block.scalar
        def _(e):
            run(e, "act")

        @block.vector
        def _(e):
            run(e, "dve")

        @block.gpsimd
        def _(e):
            run(e, "pool")

        @block.tensor
        def _(e):
            run(e, "pe")

    def mm(self, out, lhsT, rhs, start=True, stop=True):
        return self.op("pe", lambda e: e.matmul(_ap(out), _ap(lhsT), _ap(rhs), start=start, stop=stop),
                       reads=[lhsT, rhs] + ([] if start else [out]), writes=[out])

    def tr(self, out, in_, ident):
        return self.op("pe", lambda e: e.transpose(_ap(out), _ap(in_), _ap(ident)),
                       reads=[in_, ident], writes=[out])

    def act(self, out, in_, func, bias=0.0, scale=1.0, accum=None, eng="act"):
        kw = {}
        if accum is not None:
            kw["accum_out"] = _ap(accum)
        return self.op(eng, lambda e: e.activation(_ap(out), _ap(in_), func, bias=_ap(bias), scale=_ap(scale), **kw),
                       reads=[in_, bias, scale], writes=[out] + ([accum] if accum is not None else []))

    def ts(self, out, in0, s1, s2, op0, op1=ALU.bypass, eng="dve", accum=None):
        kw = {}
        if accum is not None:
            kw["accum_out"] = _ap(accum)
        return self.op(eng, lambda e: e.tensor_scalar(_ap(out), _ap(in0), _ap(s1), _ap(s2), op0, op1, **kw),
                       reads=[in0, s1, s2], writes=[out] + ([accum] if accum is not None else []))

    def tt(self, out, in0, in1, op, eng="dve"):
        return self.op(eng, lambda e: e.tensor_tensor(_ap(out), _ap(in0), _ap(in1), op),
                       reads=[in0, in1], writes=[out])

    def stt(self, out, in0, scalar, in1, op0, op1, accum=None):
        kw = {}
        if accum is not None:
            kw["accum_out"] = _ap(accum)
        return self.op("dve", lambda e: e.scalar_tensor_tensor(_ap(out), _ap(in0), _ap(scalar), _ap(in1), op0, op1, **kw),
                       reads=[in0, scalar, in1], writes=[out] + ([accum] if accum is not None else []))

    def copy(self, out, in_, eng="dve"):
        if eng == "act":
            return self.op("act", lambda e: e.copy(_ap(out), _ap(in_)), reads=[in_], writes=[out])
        return self.op(eng, lambda e: e.tensor_copy(_ap(out), _ap(in_)), reads=[in_], writes=[out])

    def memset(self, out, val, eng="pool"):
        return self.op(eng, lambda e: e.memset(_ap(out), val), reads=[], writes=[out])

    def recip(self, out, in_):
        return self.op("dve", lambda e: e.reciprocal(_ap(out), _ap(in_)), reads=[in_], writes=[out])

    def scan(self, out, d0, d1, init, op0, op1):
        return self.op("dve", lambda e: e.tensor_tensor_scan(_ap(out), _ap(d0), _ap(d1), _ap(init), op0, op1),
                       reads=[d0, d1, init], writes=[out])

    def reduce(self, out, in_, op, axis=AX.X, eng="dve"):
        return self.op(eng, lambda e: e.tensor_reduce(_ap(out), _ap(in_), axis, op), reads=[in_], writes=[out])

    def affsel(self, out, in_, pattern, cmp, fill, base, cm):
        return self.op("pool", lambda e: e.affine_select(_ap(out), _ap(in_), pattern, cmp, fill, base=base, channel_multiplier=cm),
                       reads=[in_], writes=[out])

    def iota(self, out, pattern, base, cm):
        return self.op("pool", lambda e: e.iota(_ap(out), pattern, base=base, channel_multiplier=cm), reads=[], writes=[out])

    def powm(self, out, in_, mh):
        return self.op("pool", lambda e: e.tensor_tensor(_ap(out), _ap(in_), _ap(mh), ALU.pow), reads=[in_, mh], writes=[out])

    def rsq(self, out, in_):
        self.act(out, in_, AF.Ln)
        return self.act(out, out, AF.Exp, scale=-0.5)

    def dma(self, out, in_, ring, eng="sp", **kw):
        return self.op(eng, lambda e: e.dma_start(out=_ap(out), in_=_ap(in_), **kw), reads=[in_], writes=[out], ring=ring)

import math
from concourse.bass_utils import run_bass_kernel_spmd

NT = 4096
D = 1024
TG = 2048
NG = NT // TG
SB = 128
NSB = TG // SB
LCH = 64
NCH = SB // LCH
C0 = math.exp(-0.5)
NE = 32
ARENA_KB = 176


def add_barrier(S):
    cur = []
    for e in S.engs:
        c = S.count[e]
        if c:
            cur.append((f"e_{e}_{(c - 1) // EPOCH}", (c - 1) % EPOCH + 1, e))
    for s, v in S.dmacount.items():
        cur.append((s, v, None))
    for e in S.engs:
        waits = []
        for s, v, own in cur:
            if own == e:
                continue
            if S.seen[e].get(s, 0) >= v:
                continue
            S.seen[e][s] = v
            waits.append((s, v))
        if waits:
            S.ops[e].append((None, waits, None))


class Arena:
    def __init__(self, S, kb):
        self.S = S
        self.n = kb * 256
        self.t = S.tile([128, self.n], F32, "arena")
        self.k = 0

    def at(self, off_bytes, shape, dt, name=None, parts=128):
        self.k += 1
        esz = 2 if dt == BF16 else 4
        fsz = 1
        for s in shape[1:]:
            fsz *= s
        nby = fsz * esz
        assert off_bytes % 4 == 0
        assert off_bytes + nby <= self.n * 4, (name, off_bytes, nby)
        w = (nby + 3) // 4
        ap = self.t.ap[0:shape[0], off_bytes // 4: off_bytes // 4 + w]
        if dt != F32:
            ap = ap.bitcast(dt)
        if len(shape) == 3:
            ap = ap.rearrange("p (a b) -> p a b", a=shape[1])
        elif len(shape) == 4:
            ap = ap.rearrange("p (a b c) -> p a b c", a=shape[1], b=shape[2])
        return V(ap, Unit(name or f"ar{self.k}")), off_bytes + ((nby + 3) // 4) * 4


class Bump:
    def __init__(self, arena, start_kb, end_kb):
        self.a = arena
        self.off = start_kb * 1024
        self.end = end_kb * 1024

    def get(self, shape, dt, name=None):
        v, self.off = self.a.at(self.off, shape, dt, name)
        assert self.off <= self.end, (name, self.off, self.end)
        return v


def build_program(dbg=None, n_exp=NE, stages=("mix", "moe"), upto=None, nsb=NSB):
    nc = bass.Bass("TRN2", target_bir_lowering=False)

    def din(name, shape):
        return nc.dram_tensor(name, list(shape), F32, kind="ExternalInput").ap()

    x_d = din("x", [NT, D])
    g1_d = din("norm1_g", [1, D])
    g2_d = din("norm2_g", [1, D])
    gf_d = din("final_g", [1, D])
    win_d = din("w_in", [D, 2304])
    mu_d = din("mu_c", [128, 14])
    mu64_d = din("mu64_c", [64, 24])
    w0_d = din("w0_c", [64, 8])
    a0_d = din("a0_c", [64, 8])
    kk_d = din("kk_c", [64, 8])
    ka_d = din("ka_c", [64, 8])
    rk_d = din("rk_c", [64, 8])
    lnw_d = din("lnw", [1, 512])
    lnb_d = din("lnb", [1, 512])
    waup_d = din("wa_up", [128, 512])
    gup_d = din("g_up", [128, 512])
    lre_d = din("lam_re_c", [128, 16])
    lim_d = din("lam_im_c", [128, 16])
    ls_d = din("ls_c", [128, 16])
    xbre_d = din("xb_re", [128, 16, 128])
    xbim_d = din("xb_im", [128, 16, 128])
    ctre_d = din("ct_re", [128, 16, 64])
    ctim_d = din("ct_im", [128, 16, 64])
    dsk_d = din("dsk_c", [128, 4])
    bglu_d = din("bglu_c", [128, 4])
    beta_d = din("beta_c", [128, 4])
    wglu_d = din("w_glu", [512, 512])
    wout_d = din("w_out", [D, D])
    rw_d = din("router_w", [D, NE])
    rb_d = din("router_b", [1, NE])
    w1_d = din("w1r", [NE, 2, D, D])
    b1_d = din("b1_c", [128, NE, 2, 8])
    w2_d = din("w2", [NE, D, D])
    b2_d = din("b2", [NE, D])
    out_d = nc.dram_tensor("out", [NT, D], F32, kind="ExternalOutput").ap()
    dbg_d = None
    if dbg is not None:
        dbg_d = nc.dram_tensor("dbg", list(dbg[1]), F32, kind="ExternalOutput").ap()

    with contextlib.ExitStack() as st:
        S = Sched(nc, st)
        PS = [S.psum([128, 512], F32, "psb%d" % i) for i in range(8)]
        psi = [0]

        def nextps():
            p = PS[psi[0] % 8]
            psi[0] += 1
            return p

        ident_bf = S.tile([128, 128], BF16, "ident_bf")
        ident_f = S.tile([128, 128], F32, "ident_f")
        for t in (ident_bf, ident_f):
            S.memset(t, 0.0)
            S.affsel(t, t, [[-1, 128]], ALU.not_equal, 1.0, 0, 1)
        onesblk = S.tile([128, 128], F32, "onesblk")
        S.memset(onesblk, 0.0)
        S.memset(onesblk[0:64, 0:64], 1.0)
        S.memset(onesblk[64:128, 64:128], 1.0)
        ones_f = S.tile([128, 128], F32, "ones_f")
        S.memset(ones_f, 1.0)
        mhalf = S.tile([128, 8], F32, "mhalf")
        S.memset(mhalf, -0.5)
        hsel = S.tile([128, 2], BF16, "hsel")
        S.memset(hsel, 0.0)
        S.memset(hsel[0:64, 0:1], 1.0)
        S.memset(hsel[64:128, 1:2], 1.0)
        chunkmask = S.tile([128, SB], BF16, "chunkmask")
        S.memset(chunkmask, 1.0)
        S.memset(chunkmask.re("p (c l) -> p c l", l=LCH)[:, :, 0:1], 0.0)
        MUs = S.tile([64, 512], BF16, "MUs")
        MUi = S.tile([64, 512], BF16, "MUi")
        MLs = S.tile([64, 512], BF16, "MLs")
        I8 = S.tile([64, 512], BF16, "I8")
        for t in (MUs, MUi, MLs):
            S.memset(t, 1.0)
        S.memset(I8, 0.0)
        S.affsel(MUs, MUs, [[0, 8], [1, 64]], ALU.is_gt, 0.0, 0, -1)
        S.affsel(MUi, MUi, [[0, 8], [1, 64]], ALU.is_ge, 0.0, 0, -1)
        S.affsel(MLs, MLs, [[0, 8], [-1, 64]], ALU.is_gt, 0.0, 0, 1)
        S.affsel(I8, I8, [[0, 8], [-1, 64]], ALU.not_equal, 1.0, 0, 1)

        def ptile(d, shape, name, bc=None):
            t = S.tile(shape, F32, name)
            S.dma(t, d if bc is None else d.partition_broadcast(bc), "c0")
            return t

        mu = ptile(mu_d, [128, 14], "mu")
        mu64 = ptile(mu64_d, [64, 24], "mu64")
        w0c = ptile(w0_d, [64, 8], "w0c")
        a0c = ptile(a0_d, [64, 8], "a0c")
        kkc = ptile(kk_d, [64, 8], "kkc")
        kac = ptile(ka_d, [64, 8], "kac")
        rkc = ptile(rk_d, [64, 8], "rkc")
        dsk = ptile(dsk_d, [128, 4], "dsk")
        bglu = ptile(bglu_d, [128, 4], "bglu")
        betac = ptile(beta_d, [128, 4], "betac")
        lnw_b = S.tile([64, 512], BF16, "lnw_b")
        S.dma(lnw_b, lnw_d.partition_broadcast(64), "c1", eng="pool")
        lnb_b = S.tile([64, 512], BF16, "lnb_b")
        S.dma(lnb_b, lnb_d.partition_broadcast(64), "c1", eng="pool")
        rb_b = ptile(rb_d, [128, NE], "rb_b", bc=128)
        b1c = ptile(b1_d, [128, NE, 2, 8], "b1c")
        wa_up = S.tile([128, 512], BF16, "wa_up_sb")
        S.dma(wa_up, waup_d, "c1", eng="pool")
        g_up = S.tile([128, 512], BF16, "g_up_sb")
        S.dma(g_up, gup_d, "c1", eng="pool")
        rw_sb = S.tile([128, 8, NE], BF16, "rw_sb")
        S.dma(rw_sb, rw_d.rearrange("(k p) e -> p k e", p=128), "c1", eng="pool")
        ctre = S.tile([128, 16, 64], BF16, "ctre")
        S.dma(ctre, ctre_d, "c1", eng="pool")
        nctim = S.tile([128, 16, 64], BF16, "nctim")

        Sst = S.tile([64, 8, 64], F32, "Sst")
        Sbf = S.tile([64, 8, 64], BF16, "Sbf")
        S.memset(Sst, 0.0)
        S.memset(Sbf, 0.0)
        car_re = S.tile([128, 16], F32, "car_re")
        car_im = S.tile([128, 16], F32, "car_im")
        S.memset(car_re, 0.0)
        S.memset(car_im, 0.0)
        lastcol = S.tile([128, 14], F32, "lastcol")
        S.memset(lastcol, 0.0)
        lastcol64 = S.tile([64, 24], F32, "lastcol64")
        S.memset(lastcol64, 0.0)

        AR = Arena(S, ARENA_KB)

        bt = Bump(AR, 0, ARENA_KB)
        pw_re = S.tile([128, 12, 16], F32, "pw_re")
        pw_im = S.tile([128, 12, 16], F32, "pw_im")
        npw_im = S.tile([128, 12, 16], F32, "npw_im")
        BBT_re = S.tile([128, 16, 128], BF16, "BBT_re")
        BBT_im = S.tile([128, 16, 128], BF16, "BBT_im")
        if True:
            sm = [S.tile([128, 16], F32, "s5p%d" % i) for i in range(14)]
            lre, lim, lsc, dt_, t1, ang, kf, cosv, sinv, den, zre, zim, m1, m2 = sm
            ki = S.tile([128, 16], I32, "s5ki")
            for t, d in ((lre, lre_d), (lim, lim_d), (lsc, ls_d)):
                S.dma(t, d, "c0")
            xbre = bt.get([128, 16, 128], F32, "xbre")
            xbim = bt.get([128, 16, 128], F32, "xbim")
            bbx = bt.get([128, 128], F32, "bbx")
            bbx2 = bt.get([128, 128], F32, "bbx2")
            ctim_f = bt.get([128, 16, 64], F32, "ctim_f")
            S.dma(xbre, xbre_d, "c0")
            S.dma(xbim, xbim_d, "c0")
            S.dma(ctim_f, ctim_d, "c0")
            add_barrier(S)
            S.ts(b1c[:, :, 1, :], b1c[:, :, 1, :], 1.0, None, ALU.add)
            S.ts(w0c, w0c, -1.0, None, ALU.mult)
            S.ts(a0c, a0c, -1.0, None, ALU.mult)
            S.ts(lre, lre, -1e-4, None, ALU.min)
            S.act(dt_, lsc, AF.Exp)
            S.tt(t1, lre, dt_, ALU.mult)
            mag = pw_re[:, 0, :]
            S.act(t1, t1, AF.Exp)
            S.tt(ang, lim, dt_, ALU.mult)

            def sin_of(out, shift):
                S.ts(kf, ang, 1.0 / (2 * math.pi), shift / (2 * math.pi), ALU.mult, ALU.add)
                S.copy(ki, kf)
                S.copy(m1, ki)
                S.tt(kf, kf, m1, ALU.subtract)
                S.ts(m1, kf, 0.5, None, ALU.is_gt)
                S.ts(m2, kf, -0.5, None, ALU.is_lt)
                S.tt(kf, kf, m1, ALU.subtract)
                S.tt(kf, kf, m2, ALU.add)
                S.act(out, kf, AF.Sin, scale=2 * math.pi)

            sin_of(sinv, 0.0)
            sin_of(cosv, math.pi / 2)
            S.tt(pw_re[:, 0, :], t1, cosv, ALU.mult)
            S.tt(pw_im[:, 0, :], t1, sinv, ALU.mult)
            S.tt(den, lre, lre, ALU.mult)
            S.tt(m1, lim, lim, ALU.mult)
            S.tt(den, den, m1, ALU.add)
            S.recip(den, den)
            S.ts(m1, pw_re[:, 0, :], -1.0, None, ALU.add)
            S.tt(zre, m1, lre, ALU.mult)
            S.tt(m2, pw_im[:, 0, :], lim, ALU.mult)
            S.tt(zre, zre, m2, ALU.add)
            S.tt(zre, zre, den, ALU.mult)
            S.tt(zim, pw_im[:, 0, :], lre, ALU.mult)
            S.tt(m2, m1, lim, ALU.mult)
            S.tt(zim, zim, m2, ALU.subtract)
            S.tt(zim, zim, den, ALU.mult)
            for k in range(1, 12):
                S.tt(m1, pw_re[:, k - 1, :], pw_re[:, k - 1, :], ALU.mult)
                S.tt(m2, pw_im[:, k - 1, :], pw_im[:, k - 1, :], ALU.mult)
                S.tt(pw_re[:, k, :], m1, m2, ALU.subtract)
                S.tt(m1, pw_re[:, k - 1, :], pw_im[:, k - 1, :], ALU.mult)
                S.ts(pw_im[:, k, :], m1, 2.0, None, ALU.mult)
            S.ts(npw_im, pw_im, -1.0, None, ALU.mult)
            S.act(nctim, ctim_f, AF.Copy, scale=-1.0)
            for s in range(16):
                S.ts(bbx, xbim[:, s, :], zim[:, s:s + 1], -1.0, ALU.mult, ALU.mult)
                S.stt(bbx, xbre[:, s, :], zre[:, s:s + 1], bbx, ALU.mult, ALU.add)
                ps = nextps()
                S.tr(ps[:, 0:128], bbx, ident_f)
                S.copy(BBT_re[:, s, :], ps[:, 0:128], eng="act")
                S.ts(bbx2, xbre[:, s, :], zim[:, s:s + 1], None, ALU.mult)
                S.stt(bbx2, xbim[:, s, :], zre[:, s:s + 1], bbx2, ALU.mult, ALU.add)
                ps = nextps()
                S.tr(ps[:, 0:128], bbx2, ident_f)
                S.copy(BBT_im[:, s, :], ps[:, 0:128], eng="act")
        add_barrier(S)
        if upto == 'setup':
            S.emit()
            return nc

        for g in range(NG):
            gt0 = g * TG
            mixT, _ = AR.at(0, [128, 8, TG], BF16, "mixT")
            uT, _ = AR.at(32 * 1024, [128, 4, TG], BF16, "uT")
            if "mix" in stages:
                bt = Bump(AR, 48, ARENA_KB)
                w_in_sb = bt.get([128, 8, 2304], BF16, "w_in_sb")
                S.dma(w_in_sb, win_d.rearrange("(k p) c -> p k c", p=128), "win", eng="pool")
                g1b = bt.get([128, D], F32, "g1b")
                S.dma(g1b, g1_d.partition_broadcast(128), "g1b")
                xt = [bt.get([128, D], F32, "xt0")] * 2
                hb = [bt.get([128, D], BF16, "hb%d" % i) for i in range(2)]
                hT = bt.get([128, 8, SB], BF16, "hT")
                st4 = bt.get([128, 8], F32, "st4")
                raws = [bt.get([128, SB + 2], F32, "raw%d" % i) for i in range(2)]
                rawi = [0]
                p12 = bt.get([128, SB], F32, "p12")
                p13 = bt.get([128, SB], F32, "p13")
                prkv2 = [[bt.get([64, SB], F32, "prkv%d_%d" % (i, j)) for j in range(3)] for i in range(2)]
                tw = bt.get([128, SB], BF16, "tw")
                alr = bt.get([128, SB], BF16, "alr")
                sgl = bt.get([128, SB], BF16, "sgl")
                f32t2 = [[bt.get([64, SB], F32, "wk%d_%d" % (j, i)) for i in range(12)] for j in range(2)]
                f32t = f32t2[0]
                Gl = bt.get([64, NCH, 8], F32, "Gl")
                AT = bt.get([64, 8, SB], BF16, "AT")
                RT = bt.get([64, 8, SB], BF16, "RT")
                BT = bt.get([64, 8, SB], BF16, "BT")
                KT = bt.get([64, 8, SB], BF16, "KT")
                BhT = bt.get([64, 8, SB], BF16, "BhT")
                KhT = bt.get([64, 8, SB], BF16, "KhT")
                vT = bt.get([64, 8, SB], BF16, "vT")
                rkT = bt.get([64, 8, SB], BF16, "rkT")
                Atoks = [bt.get([64, 512], BF16, "Atok%d" % i) for i in range(NCH)]
                Bhat = [bt.get([64, 512], BF16, "Bhat%d" % i) for i in range(NCH)]
                Khat = [bt.get([64, 512], BF16, "Khat%d" % i) for i in range(NCH)]
                Vtok = [bt.get([64, 512], BF16, "Vtok%d" % i) for i in range(NCH)]
                Nbr = [bt.get([64, 512], BF16, "Nbr%d" % i) for i in range(NCH)]
                Nkr = [bt.get([64, 512], BF16, "Nkr%d" % i) for i in range(NCH)]
                W2 = [bt.get([64, 512], BF16, "W2_%d" % i) for i in range(NCH)]
                ApT = [bt.get([64, 512], BF16, "ApT%d" % i) for i in range(NCH)]
                Pms = [[bt.get([64, 512], BF16, "Pm%d_%d" % (i, j)) for j in range(2)] for i in range(NCH)]
                PTms = [[bt.get([64, 512], BF16, "PTm%d_%d" % (i, j)) for j in range(2)] for i in range(NCH)]
                Tms = [[bt.get([64, 512], BF16, "Tm%d_%d" % (i, j)) for j in range(2)] for i in range(NCH)]
                MakTs = [bt.get([64, 512], BF16, "MakT%d" % i) for i in range(NCH)]
                Usb = bt.get([64, 512], BF16, "Usb")
                ysbs = [bt.get([64, 512], F32, "ysb%d" % i) for i in range(NCH)]
                yo = bt.get([64, 512], BF16, "yo")
                vb = bt.get([64, 512], F32, "vb")
                sm8 = [bt.get([64, 8], F32, "sm8_%d" % i) for i in range(6)]

                def hc(h):
                    return slice(h * 64, (h + 1) * 64)

                def emit_norm(sb_):
                    tok0_ = gt0 + sb_ * SB
                    for t2 in range(SB // 128):
                        b = t2 % 2
                        S.dma(xt[b], x_d[tok0_ + t2 * 128: tok0_ + (t2 + 1) * 128, :], "x0")
                        S.act(hb[b], xt[b], AF.Square, accum=st4[:, 0:1])
                        S.ts(st4[:, 1:2], st4[:, 0:1], 1.0 / D, 1e-5, ALU.mult, ALU.add)
                        S.rsq(st4[:, 2:3], st4[:, 1:2])
                        S.stt(hb[b], xt[b], st4[:, 2:3], g1b, ALU.mult, ALU.mult)
                        pb = nextps().bitcast(BF16)
                        for k in range(8):
                            S.tr(pb[:, k * 128:(k + 1) * 128], hb[b][:, k * 128:(k + 1) * 128], ident_bf)
                        S.copy(hT[:, :, t2 * 128:(t2 + 1) * 128], pb.re("p (k t) -> p k t", k=8), eng="act")

                emit_norm(0)
                for sb in range(nsb):
                    tok0 = gt0 + sb * SB
                    lo = sb * SB
                    for t2 in range(0):
                        b = t2 % 2
                        S.dma(xt[b], x_d[tok0 + t2 * 128: tok0 + (t2 + 1) * 128, :], "x0")
                        S.act(hb[b], xt[b], AF.Square, accum=st4[:, 0:1])
                        S.ts(st4[:, 1:2], st4[:, 0:1], 1.0 / D, 1e-5, ALU.mult, ALU.add)
                        S.rsq(st4[:, 2:3], st4[:, 1:2])
                        S.stt(hb[b], xt[b], st4[:, 2:3], g1b, ALU.mult, ALU.mult)
                        pb = nextps().bitcast(BF16)
                        for k in range(8):
                            S.tr(pb[:, k * 128:(k + 1) * 128], hb[b][:, k * 128:(k + 1) * 128], ident_bf)
                        S.copy(hT[:, :, t2 * 128:(t2 + 1) * 128], pb.re("p (k t) -> p k t", k=8), eng="act")
                    if upto == 'R_h':
                        S.emit()
                        return nc

                    def proj(c0, w):
                        ps = nextps()
                        for k in range(8):
                            S.mm(ps[0:w, 0:SB], w_in_sb[:, k, c0:c0 + w], hT[:, k, :], start=(k == 0), stop=(k == 7))
                        return ps[0:w, 0:SB]

                    def shifted(c0, w, dst, lc, muc):
                        ps = proj(c0, w)
                        r_ = raws[rawi[0] % 2][0:w, :]
                        rawi[0] += 1
                        S.copy(r_[:, 0:1], lc)
                        S.copy(r_[:, 1:SB + 1], ps, eng="act")
                        S.copy(lc, r_[:, SB:SB + 1])
                        S.tt(dst, r_[:, 0:SB], r_[:, 1:SB + 1], ALU.subtract)
                        S.stt(dst, dst, muc, r_[:, 1:SB + 1], ALU.mult, ALU.add)

                    shifted(12 * 128, 128, p12, lastcol[:, 12:13], mu[:, 12:13])
                    twt = f32t2[1][11]
                    S.act(twt, p12[0:64, :], AF.Exp, scale=-2.0)
                    S.ts(twt, twt, 1.0, None, ALU.add)
                    S.recip(twt, twt)
                    S.ts(tw[0:64, :], twt, 2.0, -1.0, ALU.mult, ALU.add)
                    S.copy(alr[64:128, :], p12[64:128, :], eng="act")
                    shifted(13 * 128, 128, p13, lastcol[:, 13:14], mu[:, 13:14])
                    S.act(p13, p13, AF.Exp, scale=-1.0)
                    S.ts(p13, p13, 1.0, None, ALU.add)
                    S.recip(p13, p13)
                    S.copy(sgl, p13, eng="act")
                    c3 = lambda t: t.re("p (c l) -> p c l", l=LCH)
                    def S1(h):
                        pr, pk, pv = prkv2[h % 2]
                        sgw, cs, Gi, Ginv, cse, Ge, Ghat, a_, kk2, nrm, kap, kmod = f32t2[h % 2]
                        shifted(h * 64, 64, pr, lastcol64[:, h:h + 1], mu64[:, h:h + 1])
                        shifted(512 + h * 64, 64, pk, lastcol64[:, 8 + h:9 + h], mu64[:, 8 + h:9 + h])
                        shifted(1024 + h * 64, 64, pv, lastcol64[:, 16 + h:17 + h], mu64[:, 16 + h:17 + h])
                        psW = nextps()
                        S.mm(psW[0:64, 0:SB], wa_up[0:64, hc(h)], tw[0:64, :])
                        S.act(sgw, psW[0:64, 0:SB], AF.Exp, bias=w0c[:, h:h + 1], scale=-1.0)
                        psA = nextps()
                        S.mm(psA[0:64, 0:SB], wa_up[64:128, hc(h)], alr[64:128, :])
                        S.act(a_, psA[0:64, 0:SB], AF.Exp, bias=a0c[:, h:h + 1], scale=-1.0)
                        S.act(kk2, pk, AF.Square, scale=kkc[:, h:h + 1])
                        psN = nextps()
                        S.mm(psN[0:64, 0:SB], ones_f[0:64, 0:64], kk2)
                        S.ts(sgw, sgw, 1.0, None, ALU.add)
                        S.recip(sgw, sgw)
                        S.scan(cs, chunkmask[0:64, :], sgw, 0.0, ALU.mult, ALU.add)
                        S.ts(nrm, psN[0:64, 0:SB], 3e-19, None, ALU.max)
                        S.act(Gi, cs, AF.Exp, scale=-C0)
                        S.act(Ginv, cs, AF.Exp, scale=C0)
                        S.tt(cse, cs, sgw, ALU.subtract)
                        S.act(Ge, cse, AF.Exp, scale=-C0)
                        S.rsq(nrm, nrm)
                        S.ts(a_, a_, 1.0, None, ALU.add)
                        S.recip(a_, a_)

                    def S2(h):
                        pr, pk, pv = prkv2[h % 2]
                        sgw, cs, Gi, Ginv, cse, Ge, Ghat, a_, kk2, nrm, kap, kmod = f32t2[h % 2]
                        S.copy(Gl[:, :, h], c3(Gi)[:, :, LCH - 1])
                        S.tt(c3(Ghat), c3(Ginv), Gl[:, :, h:h + 1].bc([64, NCH, LCH]), ALU.mult)
                        S.stt(kap, pk, kkc[:, h:h + 1], nrm, ALU.mult, ALU.mult)
                        S.ts(kmod, a_, -1.0, kac[:, h:h + 1], ALU.add, ALU.mult)
                        S.stt(kmod, kmod, 1.0, pk, ALU.add, ALU.mult)
                        S.stt(AT[:, h, :], kap, -1.0, Ge, ALU.mult, ALU.mult)
                        S.tt(RT[:, h, :], pr, Gi, ALU.mult)
                        S.tt(kap, kap, a_, ALU.mult)
                        S.tt(BT[:, h, :], kap, Ginv, ALU.mult)
                        S.tt(KT[:, h, :], kmod, Ginv, ALU.mult)
                        S.tt(BhT[:, h, :], kap, Ghat, ALU.mult)
                        S.tt(KhT[:, h, :], kmod, Ghat, ALU.mult)
                        S.copy(vT[:, h, :], pv, eng="act")
                        S.stt(rkT[:, h, :], pr, rkc[:, h:h + 1], kmod, ALU.mult, ALU.mult)

                    S1(0)
                    for h in range(8):
                        if h + 1 < 8:
                            S1(h + 1)
                        S2(h)
                    for q in range(4):
                        ps = proj(1792 + q * 128, 128)
                        S.copy(uT[:, q, lo:lo + SB], ps, eng="act")
                    if upto == 'R_prep':
                        S.emit()
                        return nc

                    for c in range(NCH):
                        cs_ = slice(c * LCH, (c + 1) * LCH)
                        for src, dst in ((AT, Atoks[c]), (BhT, Bhat[c]), (KhT, Khat[c]), (vT, Vtok[c])):
                            pb = nextps().bitcast(BF16)
                            for h in range(8):
                                S.tr(pb[0:64, hc(h)], src[:, h, cs_], ident_bf[0:64, 0:64])
                            S.copy(dst, pb[0:64, 0:512], eng="act")

                        def prod(X, Y):
                            ps = nextps()
                            for h in range(8):
                                S.mm(ps[0:64, hc(h)], X[:, h, cs_], Y[:, h, cs_])
                            return ps[0:64, :]

                        P, PT, T = Pms[c][0], PTms[c][0], Tms[c][0]
                        ps = prod(BT, AT)
                        S.tt(P, ps, MUs, ALU.mult)
                        S.tt(T, P, I8, ALU.add)
                        ps = prod(BT, RT)
                        S.tt(Nbr[c], ps, MUi, ALU.mult)
                        ps = prod(KT, RT)
                        S.tt(Nkr[c], ps, MUi, ALU.mult)
                        ps = prod(AT, KT)
                        S.tt(MakTs[c], ps, MLs, ALU.mult)
                        ps = prod(AT, BT)
                        S.tt(PT, ps, MLs, ALU.mult)
                    cur = 0
                    for lvl in range(1, 6):
                        for c in range(NCH):
                            P, PT, T = Pms[c][cur], PTms[c][cur], Tms[c][cur]
                            P2, PT2, T2 = Pms[c][1 - cur], PTms[c][1 - cur], Tms[c][1 - cur]
                            if lvl < 5:
                                psP = nextps()
                                for h in range(8):
                                    S.mm(psP[0:64, hc(h)], PT[:, hc(h)], P[:, hc(h)])
                                S.copy(P2, psP[0:64, :], eng="act")
                            psPT = nextps()
                            for h in range(8):
                                S.mm(psPT[0:64, hc(h)], P[:, hc(h)], PT[:, hc(h)])
                            S.copy(PT2, psPT[0:64, :], eng="dve")
                        for c in range(NCH):
                            T, PT2, T2 = Tms[c][cur], PTms[c][1 - cur], Tms[c][1 - cur]
                            psT = nextps()
                            for h in range(8):
                                S.mm(psT[0:64, hc(h)], PT2[:, hc(h)], T[:, hc(h)])
                            S.tt(T2, psT[0:64, :], T, ALU.add)
                        cur = 1 - cur
                    for c in range(NCH):
                        T = Tms[c][cur]
                        psA = nextps()
                        for h in range(8):
                            S.mm(psA[0:64, hc(h)], Atoks[c][:, hc(h)], T[:, hc(h)])
                        S.copy(ApT[c], psA[0:64, :], eng="act")
                        psW = nextps()
                        for h in range(8):
                            S.mm(psW[0:64, hc(h)], MakTs[c][:, hc(h)], T[:, hc(h)])
                        S.copy(W2[c], psW[0:64, :], eng="dve")
                    if upto == 'R_chunk':
                        S.emit()
                        return nc

                    if sb + 1 < nsb:
                        emit_norm(sb + 1)
                    for c in range(NCH):
                        cs_ = slice(c * LCH, (c + 1) * LCH)
                        psU = nextps()
                        for h in range(8):
                            S.mm(psU[0:64, hc(h)], ApT[c][:, hc(h)], Sbf[:, h, :], start=True, stop=False)
                            S.mm(psU[0:64, hc(h)], W2[c][:, hc(h)], Vtok[c][:, hc(h)], start=False, stop=True)
                        S.copy(Usb, psU[0:64, :], eng="act")
                        psY = nextps()
                        for h in range(8):
                            S.mm(psY[0:64, hc(h)], RT[:, h, cs_], Sbf[:, h, :], start=True, stop=False)
                            S.mm(psY[0:64, hc(h)], Nbr[c][:, hc(h)], Usb[:, hc(h)], start=False, stop=False)
                            S.mm(psY[0:64, hc(h)], Nkr[c][:, hc(h)], Vtok[c][:, hc(h)], start=False, stop=True)
                        psS = nextps()
                        for h in range(8):
                            S.mm(psS[0:64, hc(h)], Bhat[c][:, hc(h)], Usb[:, hc(h)], start=True, stop=False)
                            S.mm(psS[0:64, hc(h)], Khat[c][:, hc(h)], Vtok[c][:, hc(h)], start=False, stop=True)
                        S.tt(Sst, Sst, Gl[:, c, :].re("p (a o) -> p a o", o=1).bc([64, 8, 64]), ALU.mult)
                        S.tt(Sst, Sst, psS[0:64, :].re("p (a v) -> p a v", a=8), ALU.add)
                        S.copy(Sbf, Sst, eng="act")
                        S.copy(ysbs[c], psY[0:64, :], eng="act")
                    for c in range(NCH):
                        cs_ = slice(c * LCH, (c + 1) * LCH)
                        ysb = ysbs[c]
                        s1, s2, mn, var, rs, bon = sm8
                        y3 = lambda t: t.re("p (h n) -> p h n", h=8)
                        b8 = lambda t: t.re("p (h o) -> p h o", o=1).bc([64, 8, 64])
                        if upto == 'R' and g == 0 and sb == 0 and c == 0:
                            S.dma(dbg_d[0:64, 7, 0:512], ysb, "dbg2")
                        for h in range(8):
                            S.act(vb[:, hc(h)], ysb[:, hc(h)], AF.Identity, accum=s1[:, h:h + 1])
                            S.act(vb[:, hc(h)], ysb[:, hc(h)], AF.Square, accum=s2[:, h:h + 1])
                        S.ts(mn, s1, 1.0 / 64, None, ALU.mult)
                        S.tt(var, mn, mn, ALU.mult)
                        S.stt(var, s2, 1.0 / 64, var, ALU.mult, ALU.subtract)
                        S.ts(var, var, 64e-5, None, ALU.add)
                        S.rsq(rs, var)
                        S.tt(mn, mn, rs, ALU.mult)
                        S.ts(mn, mn, -1.0, None, ALU.mult)
                        for h in range(8):
                            S.act(ysb[:, hc(h)], ysb[:, hc(h)], AF.Identity, bias=mn[:, h:h + 1], scale=rs[:, h:h + 1])
                        if upto == 'R' and g == 0 and sb == 0 and c == 0:
                            for i_, t_ in enumerate(sm8[:5]):
                                S.dma(dbg_d[0:64, 7, 512 + 8 * i_: 520 + 8 * i_], t_, "dbg2")
                            S.dma(dbg_d[0:64, 7, 1024:1536], ysb, "dbg2")
                        S.tt(ysb, ysb, lnw_b, ALU.mult)
                        S.tt(ysb, ysb, lnb_b, ALU.add)
                        psB = nextps()
                        for h in range(8):
                            S.mm(psB[0:64, h:h + 1], rkT[:, h, cs_], hsel[0:64, 0:1])
                        S.copy(bon, psB[0:64, 0:8], eng="act")
                        S.tt(y3(vb), y3(Vtok[c]), b8(bon), ALU.mult)
                        S.tt(ysb, ysb, vb, ALU.add)
                        psG = nextps()
                        S.mm(psG[0:64, :], sgl[:, cs_], g_up)
                        S.tt(yo, ysb, psG[0:64, :], ALU.mult)
                        pb = nextps().bitcast(BF16)
                        for hp in range(4):
                            S.tr(pb[:, hp * 64:(hp + 1) * 64], yo[:, hp * 128:(hp + 1) * 128], ident_bf[0:64, 0:64])
                        S.copy(mixT[:, 0:4, lo + c * LCH: lo + (c + 1) * LCH], pb[:, 0:256].re("p (a t) -> p a t", a=4), eng="act")
                add_barrier(S)
                if upto == 'R':
                    dsb, _ = AR.at(48 * 1024, [128, 8, TG], F32, 'dbgsb')
                    S.copy(dsb, mixT)
                    S.dma(dbg_d[:, 0:7, :], dsb[:, 0:7, :], 'dbg')
                    S.emit()
                    return nc

                bt = Bump(AR, 48, ARENA_KB)
                A = [[bt.get([128, TG], F32, "s5A%d%d" % (i, j)) for j in range(2)] for i in range(2)]
                sbf = [bt.get([128, TG], BF16, "s5sbf%d" % i) for i in range(2)]
                sbfP = [bt.get([128, TG], BF16, "s5sbfP%d" % i) for i in range(2)]
                ptmp = bt.get([128, TG], F32, "s5ptmp")
                yTq = bt.get([128, TG], F32, "yTq")
                geT = bt.get([128, 4, TG], BF16, "geT")
                glo_off = bt.off
                glo = bt.get([128, 4, TG], F32, "glo")
                Ap = [[AR.at(glo_off + (i * 2 + j) * TG * 4, [128, TG], F32, "s5Ap%d%d" % (i, j))[0] for j in range(2)] for i in range(2)]
                wk = [bt.get([128, 512], F32, "s5wk%d" % i) for i in range(3)]
                wglu = bt.get([128, 4, 512], BF16, "wglu_sb")
                S.dma(wglu, wglu_d.rearrange("(k p) c -> p k c", p=128), "wglu", eng="pool")
                nblk = TG // 512

                def load_bu(s, dst):
                    q = s // 4
                    for blk in range(nblk):
                        bs = slice(blk * 512, (blk + 1) * 512)
                        ps = nextps()
                        S.mm(ps, BBT_re[:, s, :], uT[:, q, bs])
                        S.copy(dst[0][:, bs], ps, eng="act")
                        ps = nextps()
                        S.mm(ps, BBT_im[:, s, :], uT[:, q, bs])
                        S.copy(dst[1][:, bs], ps, eng="act")
                    S.stt(dst[0][:, 0:1], car_re[:, s:s + 1], pw_re[:, 0, s:s + 1], dst[0][:, 0:1], ALU.mult, ALU.add)
                    S.stt(dst[0][:, 0:1], car_im[:, s:s + 1], npw_im[:, 0, s:s + 1], dst[0][:, 0:1], ALU.mult, ALU.add)
                    S.stt(dst[1][:, 0:1], car_im[:, s:s + 1], pw_re[:, 0, s:s + 1], dst[1][:, 0:1], ALU.mult, ALU.add)
                    S.stt(dst[1][:, 0:1], car_re[:, s:s + 1], pw_im[:, 0, s:s + 1], dst[1][:, 0:1], ALU.mult, ALU.add)

                def doubling_dve(s, bufs):
                    cur, k, d = 0, 0, 1
                    while d < TG:
                        a, b = bufs[cur], bufs[1 - cur]
                        ar, ai, nai = pw_re[:, k, s:s + 1], pw_im[:, k, s:s + 1], npw_im[:, k, s:s + 1]
                        S.stt(b[0][:, d:], a[0][:, 0:TG - d], ar, a[0][:, d:], ALU.mult, ALU.add)
                        S.stt(b[0][:, d:], a[1][:, 0:TG - d], nai, b[0][:, d:], ALU.mult, ALU.add)
                        S.stt(b[1][:, d:], a[1][:, 0:TG - d], ar, a[1][:, d:], ALU.mult, ALU.add)
                        S.stt(b[1][:, d:], a[0][:, 0:TG - d], ai, b[1][:, d:], ALU.mult, ALU.add)
                        S.copy(b[0][:, 0:d], a[0][:, 0:d], eng="act")
                        S.copy(b[1][:, 0:d], a[1][:, 0:d], eng="act")
                        cur, d, k = 1 - cur, d * 2, k + 1
                    return cur

                def doubling_pool(s, bufs):
                    cur, k, d = 0, 0, 1
                    while d < TG:
                        a, b = bufs[cur], bufs[1 - cur]
                        ar, ai, nai = pw_re[:, k, s:s + 1], pw_im[:, k, s:s + 1], npw_im[:, k, s:s + 1]
                        n = TG - d
                        S.ts(ptmp[:, 0:n], a[0][:, 0:n], ar, 0.0, ALU.mult, ALU.add, eng="pool")
                        S.tt(b[0][:, d:], ptmp[:, 0:n], a[0][:, d:], ALU.add, eng="pool")
                        S.ts(ptmp[:, 0:n], a[1][:, 0:n], nai, 0.0, ALU.mult, ALU.add, eng="pool")
                        S.tt(b[0][:, d:], b[0][:, d:], ptmp[:, 0:n], ALU.add, eng="pool")
                        S.ts(ptmp[:, 0:n], a[1][:, 0:n], ar, 0.0, ALU.mult, ALU.add, eng="pool")
                        S.tt(b[1][:, d:], ptmp[:, 0:n], a[1][:, d:], ALU.add, eng="pool")
                        S.ts(ptmp[:, 0:n], a[0][:, 0:n], ai, 0.0, ALU.mult, ALU.add, eng="pool")
                        S.tt(b[1][:, d:], b[1][:, d:], ptmp[:, 0:n], ALU.add, eng="pool")
                        S.copy(b[0][:, 0:d], a[0][:, 0:d], eng="pool")
                        S.copy(b[1][:, 0:d], a[1][:, 0:d], eng="pool")
                        cur, d, k = 1 - cur, d * 2, k + 1
                    return cur

                def finish(s, fin, sb_):
                    q, r4 = s // 4, s % 4
                    S.copy(car_re[:, s:s + 1], fin[0][:, TG - 1:TG])
                    S.copy(car_im[:, s:s + 1], fin[1][:, TG - 1:TG])
                    S.copy(sb_[0], fin[0], eng="act")
                    S.copy(sb_[1], fin[1], eng="act")
                    for blk in range(nblk):
                        bs = slice(blk * 512, (blk + 1) * 512)
                        ps = nextps()
                        pb0 = (r4 // 2) * 64
                        o = ps[pb0:pb0 + 64, :]
                        S.mm(o, ctre[:, s, :], sb_[0][:, bs], start=True, stop=False)
                        S.mm(o, nctim[:, s, :], sb_[1][:, bs], start=False, stop=True)
                        if r4 % 2 == 0:
                            S.copy(yTq[pb0:pb0 + 64, bs], o, eng="act")
                        else:
                            S.tt(yTq[pb0:pb0 + 64, bs], yTq[pb0:pb0 + 64, bs], o, ALU.add)

                for q in range(4):
                    sP = 4 * q + 3
                    load_bu(sP, Ap[0])
                    curP = doubling_pool(sP, Ap)
                    for s in (4 * q, 4 * q + 1, 4 * q + 2):
                        load_bu(s, A[0])
                        cur = doubling_dve(s, A)
                        finish(s, A[cur], sbf)
                    finish(sP, Ap[curP], sbfP)
                    for blk in range(nblk):
                        bs = slice(blk * 512, (blk + 1) * 512)
                        yv, x2_, t_ = wk
                        S.stt(yv, uT[:, q, bs], dsk[:, q:q + 1], yTq[:, bs], ALU.mult, ALU.add)
                        S.act(x2_, yv, AF.Square)
                        S.ts(x2_, x2_, 0.044715, 1.0, ALU.mult, ALU.add)
                        S.tt(x2_, x2_, yv, ALU.mult)
                        S.act(t_, x2_, AF.Sigmoid, scale=2.0 * math.sqrt(2.0 / math.pi))
                        S.tt(geT[:, q, bs], yv, t_, ALU.mult)
                add_barrier(S)
                for blk in range(nblk):
                    bs = slice(blk * 512, (blk + 1) * 512)
                    psn = nextps()
                    for q in range(4):
                        ps = nextps()
                        for k in range(4):
                            S.mm(ps, wglu[:, k, q * 128:(q + 1) * 128], geT[:, k, bs], start=(k == 0), stop=(k == 3))
                        gl_, sq_, _ = wk
                        S.act(gl_, ps, AF.Sigmoid, bias=bglu[:, q:q + 1])
                        S.tt(glo[:, q, bs], geT[:, q, bs], gl_, ALU.mult)
                        S.act(sq_, glo[:, q, bs], AF.Square)
                        S.mm(psn, ones_f, sq_, start=(q == 0), stop=(q == 3))
                    rst = wk[2]
                    S.ts(rst, psn, 1.0 / 512, 1e-5, ALU.mult, ALU.add)
                    S.act(rst, rst, AF.Sqrt)
                    S.recip(rst, rst)
                    for q in range(4):
                        S.stt(mixT[:, 4 + q, bs], glo[:, q, bs], betac[:, q:q + 1], rst, ALU.mult, ALU.mult)
                add_barrier(S)

            if dbg is not None and dbg[0] == "mixT" and g == 0:
                dsb, _ = AR.at(48 * 1024, [128, 8, TG], F32, "dbgsb")
                S.copy(dsb, mixT)
                S.dma(dbg_d, dsb, "dbg")
                add_barrier(S)

            X2, _ = AR.at(48 * 1024, [128, 16, D], F32, "X2")
            h2T, _ = AR.at(112 * 1024, [128, 8, TG], BF16, "h2T")
            bt = Bump(AR, 144, ARENA_KB)
            wout = bt.get([128, 8, D], BF16, "wout_sb")
            S.dma(wout, wout_d.rearrange("(k p) c -> p k c", p=128), "win", eng="pool")
            g2b = bt.get([128, D], F32, "g2b")
            S.dma(g2b, g2_d.partition_broadcast(128), "c0")
            xt = [bt.get([128, D], F32, "xt2_0")] * 2
            hb = [bt.get([128, D], BF16, "hb2_%d" % i) for i in range(2)]
            b2_sb = bt.get([NE, D], F32, "b2_sb")
            S.dma(b2_sb, b2_d, "b2")
            COMB, _ = AR.at(32 * 1024, [128, 16, NE], F32, "COMB")
            COMBS, _ = AR.at(34 * 1024, [128, 16, NE], F32, "COMBS")
            combT, _ = AR.at(36 * 1024, [NE, TG], F32, "combT")
            rt = [AR.at((44 * 1024) + i * 128, [128, NE], F32, "rt%d" % i)[0] for i in range(3)]
            rs8 = AR.at(44 * 1024 + 512, [128, 16], F32, "rs8")[0]
            for t2 in range(16):
                b = t2 % 2
                ts_ = slice(t2 * 128, (t2 + 1) * 128)
                S.dma(xt[b], x_d[gt0 + t2 * 128: gt0 + (t2 + 1) * 128, :], "x0")
                for half in range(2):
                    ps = nextps()
                    for k in range(8):
                        S.mm(ps, mixT[:, k, ts_], wout[:, k, half * 512:(half + 1) * 512], start=(k == 0), stop=(k == 7))
                    S.tt(X2[:, t2, half * 512:(half + 1) * 512], xt[b][:, half * 512:(half + 1) * 512], ps, ALU.add)
                S.act(hb[b], X2[:, t2, :], AF.Square, accum=rs8[:, 0:1])
                S.ts(rs8[:, 1:2], rs8[:, 0:1], 1.0 / D, 1e-5, ALU.mult, ALU.add)
                S.rsq(rs8[:, 2:3], rs8[:, 1:2])
                S.stt(hb[b], X2[:, t2, :], rs8[:, 2:3], g2b, ALU.mult, ALU.mult)
                pb = nextps().bitcast(BF16)
                for k in range(8):
                    S.tr(pb[:, k * 128:(k + 1) * 128], hb[b][:, k * 128:(k + 1) * 128], ident_bf)
                S.copy(h2T[:, :, ts_], pb.re("p (k t) -> p k t", k=8), eng="act")
                ps = nextps()
                for k in range(8):
                    S.mm(ps[:, 0:NE], h2T[:, k, ts_], rw_sb[:, k, :], start=(k == 0), stop=(k == 7))
                lg, ex, mk = rt
                S.tt(lg, ps[:, 0:NE], rb_b, ALU.add)
                S.op("dve", lambda e, o=rs8[:, 8:16], i=lg: e.max(o.ap, i.ap), reads=[lg], writes=[rs8])
                S.ts(mk, lg, rs8[:, 11:12], None, ALU.is_ge)
                S.ts(rs8[:, 3:4], rs8[:, 8:9], -1.0, None, ALU.mult)
                S.act(ex, lg, AF.Exp, bias=rs8[:, 3:4])
                S.tt(ex, ex, mk, ALU.mult)
                S.reduce(rs8[:, 4:5], ex, ALU.add)
                S.recip(rs8[:, 5:6], rs8[:, 4:5])
                S.ts(COMB[:, t2, :], ex, rs8[:, 5:6], None, ALU.mult)
                S.ts(COMBS[:, t2, :], COMB[:, t2, :], 1.0 / 1.702, None, ALU.mult)
                ps = nextps()
                S.tr(ps[0:NE, 0:128], COMB[:, t2, :], ident_f)
                S.copy(combT[:, ts_], ps[0:NE, 0:128], eng="act")
                for half in range(2):
                    ps = nextps()
                    S.mm(ps, combT[:, ts_], b2_sb[:, half * 512:(half + 1) * 512])
                    S.tt(X2[:, t2, half * 512:(half + 1) * 512], X2[:, t2, half * 512:(half + 1) * 512], ps, ALU.add)
            add_barrier(S)

            if dbg is not None and dbg[0] == "X2" and g == 0:
                S.dma(dbg_d, X2, "dbg")
                add_barrier(S)

            if "moe" in stages:
                actT, _ = AR.at(0, [128, 8, TG], BF16, "actT")
                w2b = [AR.at((144 + 16 * i) * 1024, [128, 8, D], BF16, "w2b%d" % i)[0] for i in range(2)]
                w1b = [AR.at(36 * 1024 + i * 4096, [128, 2, 8, 128], BF16, "w1b%d" % i)[0] for i in range(2)]
                ew = [AR.at(32 * 1024 + i * 1024, [128, 512], BF16, "ew%d" % i)[0] for i in range(2)]
                exg, _ = AR.at(46 * 1024, [128, 512], F32, "exg")
                exl, _ = AR.at(44 * 1024, [128, 512], F32, "exl")
                nf = 0
                for e in range(n_exp):
                    wb = w2b[e % 2]
                    S.dma(wb, w2_d[e].rearrange("(k p) c -> p k c", p=128), "w2_%d" % (e % 2), eng="pool")
                    for f in range(8):
                        w1 = w1b[nf % 2]
                        S.dma(w1, w1_d[e, :, :, f * 128:(f + 1) * 128].rearrange("g (k p) m -> p g k m", p=128),
                              "w1_%d" % (nf % 2), eng="pool")
                        nf += 1
                        for tb in range(TG // 512):
                            bs = slice(tb * 512, (tb + 1) * 512)
                            pg = nextps()
                            for k in range(8):
                                S.mm(pg, w1[:, 0, k, :], h2T[:, k, bs], start=(k == 0), stop=(k == 7))
                            pl = nextps()
                            for k in range(8):
                                S.mm(pl, w1[:, 1, k, :], h2T[:, k, bs], start=(k == 0), stop=(k == 7))
                            S.ts(exg, pg, b1c[:, e, 0, f:f + 1], 7.0, ALU.add, ALU.min)
                            S.act(ew[0], exg, AF.Silu, scale=1.702)
                            S.act(exl, pl, AF.Identity, bias=b1c[:, e, 1, f:f + 1])
                            S.ts(ew[1], exl, -6.0, 8.0, ALU.max, ALU.min)
                            S.tt(actT[:, f, bs], ew[0], ew[1], ALU.mult)
                    for t2 in range(16):
                        ts_ = slice(t2 * 128, (t2 + 1) * 128)
                        for half in range(2):
                            ps = nextps()
                            for f in range(8):
                                S.mm(ps, actT[:, f, ts_], wb[:, f, half * 512:(half + 1) * 512], start=(f == 0), stop=(f == 7))
                            xs = X2[:, t2, half * 512:(half + 1) * 512]
                            S.stt(xs, ps, COMBS[:, t2, e:e + 1], xs, ALU.mult, ALU.add)
                add_barrier(S)

            gfb, _ = AR.at(0, [128, D], F32, "gfb")
            ob = [AR.at(4096 * (1 + i), [128, D], F32, "ob%d" % i)[0] for i in range(2)]
            rs8 = AR.at(4096 * 4, [128, 16], F32, "rs8f")[0]
            S.dma(gfb, gf_d.partition_broadcast(128), "c0")
            for t2 in range(16):
                b = t2 % 2
                S.act(ob[b], X2[:, t2, :], AF.Square, accum=rs8[:, 0:1])
                S.ts(rs8[:, 1:2], rs8[:, 0:1], 1.0 / D, 1e-5, ALU.mult, ALU.add)
                S.rsq(rs8[:, 2:3], rs8[:, 1:2])
                S.stt(ob[b], X2[:, t2, :], rs8[:, 2:3], gfb, ALU.mult, ALU.mult)
                S.dma(out_d[gt0 + t2 * 128: gt0 + (t2 + 1) * 128, :], ob[b], "out%d" % b)
            add_barrier(S)
        S.emit()
    return nc


def _prep_inputs(inp):
    f = lambda a: np.ascontiguousarray(np.asarray(a, dtype=np.float32))
    col = lambda v, m: f(np.asarray(v).reshape(m, 128).T)
    d = {}
    d["norm1_g"] = f(inp["norm1_g"][0][None])
    d["norm2_g"] = f(inp["norm2_g"][0][None])
    d["final_g"] = f(np.asarray(inp["final_g"])[None])
    d["w_in"] = f(inp["w_in"][0])
    d["mu_c"] = col(inp["mu_shift"][0], 14)
    col64 = lambda v, m: f(np.asarray(v).reshape(m, 64).T)
    d["mu64_c"] = col64(np.asarray(inp["mu_shift"][0])[:1536], 24)
    d["w0_c"] = col64(inp["w0"][0], 8)
    d["a0_c"] = col64(inp["a0"][0], 8)
    d["kk_c"] = col64(inp["k_k"][0], 8)
    d["ka_c"] = col64(inp["k_a"][0], 8)
    d["rk_c"] = col64(np.asarray(inp["r_k"][0]).reshape(-1), 8)
    d["lnw"] = f(inp["ln_x_w"][0][None])
    d["lnb"] = f(inp["ln_x_b"][0][None])
    d["wa_up"] = f(np.concatenate([inp["w_up"][0], inp["a_up"][0]], 0))
    d["g_up"] = f(inp["g_up"][0])
    d["lam_re_c"] = col(inp["lambda_re"][0].reshape(-1), 16)
    d["lam_im_c"] = col(inp["lambda_im"][0].reshape(-1), 16)
    d["ls_c"] = col(np.repeat(np.asarray(inp["log_step"][0]), 64), 16)
    for nm, src in (("xb_re", inp["b_re"][0]), ("xb_im", inp["b_im"][0])):
        src = np.asarray(src)
        xb = np.zeros((128, 16, 128), np.float32)
        for s in range(16):
            for gl in range(2):
                c0 = 32 * (s % 4) + gl * 16
                xb[gl * 64:(gl + 1) * 64, s, c0:c0 + 16] = src[2 * s + gl]
        d[nm] = xb
    for nm, src in (("ct_re", inp["c_re"][0]), ("ct_im", inp["c_im"][0])):
        src = np.asarray(src)
        ct = np.zeros((128, 16, 64), np.float32)
        for s in range(16):
            for gl in range(2):
                c0 = (s % 2) * 32 + gl * 16
                ct[gl * 64:(gl + 1) * 64, s, c0:c0 + 16] = src[2 * s + gl].T
        d[nm] = ct
    d["dsk_c"] = col(inp["d_skip"][0], 4)
    d["bglu_c"] = col(inp["b_glu"][0], 4)
    d["beta_c"] = col(inp["beta_ssm"][0], 4)
    d["w_glu"] = f(inp["w_glu"][0])
    d["w_out"] = f(inp["w_out"][0])
    d["router_w"] = f(inp["router_w"][0])
    d["router_b"] = f(inp["router_b"][0][None])
    w1 = np.asarray(inp["w1"][0])
    d["w1r"] = f(w1.reshape(NE, D, D, 2).transpose(0, 3, 1, 2))
    d["b1_c"] = f(np.asarray(inp["b1"][0]).reshape(NE, 8, 128, 2).transpose(2, 0, 3, 1))
    d["w2"] = f(inp["w2"][0])
    d["b2"] = f(inp["b2"][0])
    return d


def kernel(**inputs):
    shared = _prep_inputs(inputs)
    x = np.asarray(inputs["x"], dtype=np.float32)
    nc = build_program()
    in_maps = []
    for c in range(8):
        m = dict(shared)
        m["x"] = np.ascontiguousarray(x[c])
        in_maps.append(m)
    res = run_bass_kernel_spmd(nc, in_maps, core_ids=list(range(8)))
    return np.stack([np.asarray(r["out"]) for r in res.results], 0).astype(np.float32)
```

```python
import contextlib
import numpy as np
import concourse.bass as bass
import concourse.mybir as mybir

F32 = mybir.dt.float32
BF16 = mybir.dt.bfloat16
I32 = mybir.dt.int32
AF = mybir.ActivationFunctionType
ALU = mybir.AluOpType
AX = mybir.AxisListType

EPOCH = 30000
SAME_ENG_SYNC = True


class Unit:
    __slots__ = ("name", "w", "r")

    def __init__(self, name):
        self.name = name
        self.w = None
        self.r = {}


class V:
    __slots__ = ("ap", "u")

    def __init__(self, ap, u):
        self.ap = ap
        self.u = u

    def __getitem__(self, k):
        return V(self.ap[k], self.u)

    def re(self, pat, **kw):
        return V(self.ap.rearrange(pat, **kw), self.u)

    def bc(self, shape):
        return V(self.ap.to_broadcast(shape), self.u)

    def bitcast(self, dt):
        return V(self.ap.bitcast(dt), self.u)


def _ap(x):
    return x.ap if isinstance(x, V) else x


class Sched:
    def __init__(self, nc, stack):
        self.nc = nc
        self.stack = stack
        self.engs = {"pe": nc.tensor, "act": nc.scalar, "dve": nc.vector, "pool": nc.gpsimd, "sp": nc.sync}
        self.ops = {e: [] for e in self.engs}
        self.count = {e: 0 for e in self.engs}
        self.seen = {e: {} for e in self.engs}
        self.dmacount = {}
        self.semnames = []
        self.ntile = 0

    def tile(self, shape, dt, name=None):
        self.ntile += 1
        name = name or f"t{self.ntile}"
        t = self.stack.enter_context(self.nc.sbuf_tensor(name, list(shape), dt))
        return V(t.ap() if hasattr(t, "ap") and callable(t.ap) else t[:], Unit(name))

    def psum(self, shape, dt, name=None):
        self.ntile += 1
        name = name or f"p{self.ntile}"
        t = self.stack.enter_context(self.nc.psum_tensor(name, list(shape), dt))
        return V(t.ap() if hasattr(t, "ap") and callable(t.ap) else t[:], Unit(name))

    def op(self, eng, fn, reads=(), writes=(), ring=None):
        waits = {}
        seen = self.seen[eng]

        def need(tok, kind="raw"):
            if tok is None:
                return
            sem, val, teng, is_dma = tok
            if teng == eng and not is_dma and (not SAME_ENG_SYNC or eng == 'pe' or (kind != "raw" and eng in ("dve", "act"))):
                return
            if seen.get(sem, 0) >= val:
                return
            if waits.get(sem, 0) < val:
                waits[sem] = val

        for x in reads:
            if isinstance(x, V) and x.u is not None:
                need(x.u.w)
        for x in writes:
            if isinstance(x, V) and x.u is not None:
                need(x.u.w, "waw")
                for t in x.u.r.values():
                    need(t, "war")
        for s, v in waits.items():
            seen[s] = v
        if ring is not None:
            sem = "d_" + ring
            self.dmacount[sem] = self.dmacount.get(sem, 0) + 16
            tok = (sem, self.dmacount[sem], eng, True)
        else:
            c = self.count[eng]
            self.count[eng] = c + 1
            sem = f"e_{eng}_{c // EPOCH}"
            tok = (sem, c % EPOCH + 1, eng, False)
        if sem not in self.semnames:
            self.semnames.append(sem)
        for s in waits:
            assert s in self.semnames
        self.ops[eng].append((fn, list(waits.items()), tok))
        for x in reads:
            if isinstance(x, V) and x.u is not None:
                old = x.u.r.get(sem)
                if old is None or old[1] < tok[1]:
                    x.u.r[sem] = tok
        for x in writes:
            if isinstance(x, V) and x.u is not None:
                x.u.w = tok
                x.u.r = {}
        return tok

    def emit(self, final_waits_eng="sp"):
        nc = self.nc
        sems = {}
        for n in self.semnames:
            sems[n] = self.stack.enter_context(nc.semaphore(n))
        finals = []
        for e in self.engs:
            c = self.count[e]
            if c:
                finals.append((f"e_{e}_{(c - 1) // EPOCH}", (c - 1) % EPOCH + 1))
        for s, v in self.dmacount.items():
            finals.append((s, v))
        block = self.stack.enter_context(nc.Block())
        ops = self.ops

        def run(eng_handle, name):
            for fn, waits, tok in ops[name]:
                for s, v in waits:
                    eng_handle.wait_ge(sems[s], v)
                if fn is None:
                    continue
                ins = fn(eng_handle)
                ins.then_inc(sems[tok[0]], 16 if tok[3] else 1)
            if name == final_waits_eng:
                for s, v in finals:
                    eng_handle.wait_ge(sems[s], v)

        @block.sync
        def _(e):
            run(e, "sp")

        @block.scalar
        def _(e):
            run(e, "act")

        @block.vector
        def _(e):
            run(e, "dve")

        @block.gpsimd
        def _(e):
            run(e, "pool")

        @block.tensor
        def _(e):
            run(e, "pe")

    def mm(self, out, lhsT, rhs, start=True, stop=True):
        return self.op("pe", lambda e: e.matmul(_ap(out), _ap(lhsT), _ap(rhs), start=start, stop=stop),
                       reads=[lhsT, rhs] + ([] if start else [out]), writes=[out])

    def tr(self, out, in_, ident):
        return self.op("pe", lambda e: e.transpose(_ap(out), _ap(in_), _ap(ident)),
                       reads=[in_, ident], writes=[out])

    def act(self, out, in_, func, bias=0.0, scale=1.0, accum=None, eng="act"):
        kw = {}
        if accum is not None:
            kw["accum_out"] = _ap(accum)
        return self.op(eng, lambda e: e.activation(_ap(out), _ap(in_), func, bias=_ap(bias), scale=_ap(scale), **kw),
                       reads=[in_, bias, scale], writes=[out] + ([accum] if accum is not None else []))

    def ts(self, out, in0, s1, s2, op0, op1=ALU.bypass, eng="dve", accum=None):
        kw = {}
        if accum is not None:
            kw["accum_out"] = _ap(accum)
        return self.op(eng, lambda e: e.tensor_scalar(_ap(out), _ap(in0), _ap(s1), _ap(s2), op0, op1, **kw),
                       reads=[in0, s1, s2], writes=[out] + ([accum] if accum is not None else []))

    def tt(self, out, in0, in1, op, eng="dve"):
        return self.op(eng, lambda e: e.tensor_tensor(_ap(out), _ap(in0), _ap(in1), op),
                       reads=[in0, in1], writes=[out])

    def stt(self, out, in0, scalar, in1, op0, op1, accum=None):
        kw = {}
        if accum is not None:
            kw["accum_out"] = _ap(accum)
        return self.op("dve", lambda e: e.scalar_tensor_tensor(_ap(out), _ap(in0), _ap(scalar), _ap(in1), op0, op1, **kw),
                       reads=[in0, scalar, in1], writes=[out] + ([accum] if accum is not None else []))

    def copy(self, out, in_, eng="dve"):
        if eng == "act":
            return self.op("act", lambda e: e.copy(_ap(out), _ap(in_)), reads=[in_], writes=[out])
        return self.op(eng, lambda e: e.tensor_copy(_ap(out), _ap(in_)), reads=[in_], writes=[out])

    def memset(self, out, val, eng="pool"):
        return self.op(eng, lambda e: e.memset(_ap(out), val), reads=[], writes=[out])

    def recip(self, out, in_):
        return self.op("dve", lambda e: e.reciprocal(_ap(out), _ap(in_)), reads=[in_], writes=[out])

    def scan(self, out, d0, d1, init, op0, op1):
        return self.op("dve", lambda e: e.tensor_tensor_scan(_ap(out), _ap(d0), _ap(d1), _ap(init), op0, op1),
                       reads=[d0, d1, init], writes=[out])

    def reduce(self, out, in_, op, axis=AX.X, eng="dve"):
        return self.op(eng, lambda e: e.tensor_reduce(_ap(out), _ap(in_), axis, op), reads=[in_], writes=[out])

    def affsel(self, out, in_, pattern, cmp, fill, base, cm):
        return self.op("pool", lambda e: e.affine_select(_ap(out), _ap(in_), pattern, cmp, fill, base=base, channel_multiplier=cm),
                       reads=[in_], writes=[out])

    def iota(self, out, pattern, base, cm):
        return self.op("pool", lambda e: e.iota(_ap(out), pattern, base=base, channel_multiplier=cm), reads=[], writes=[out])

    def powm(self, out, in_, mh):
        return self.op("pool", lambda e: e.tensor_tensor(_ap(out), _ap(in_), _ap(mh), ALU.pow), reads=[in_, mh], writes=[out])

    def rsq(self, out, in_):
        self.act(out, in_, AF.Ln)
        return self.act(out, out, AF.Exp, scale=-0.5)

    def dma(self, out, in_, ring, eng="sp", **kw):
        return self.op(eng, lambda e: e.dma_start(out=_ap(out), in_=_ap(in_), **kw), reads=[in_], writes=[out], ring=ring)

import math
from concourse.bass_utils import run_bass_kernel_spmd

NT = 4096
D = 1024
TG = 2048
NG = NT // TG
SB = 128
NSB = TG // SB
LCH = 64
NCH = SB // LCH
C0 = math.exp(-0.5)
NE = 32
ARENA_KB = 176


def add_barrier(S):
    cur = []
    for e in S.engs:
        c = S.count[e]
        if c:
            cur.append((f"e_{e}_{(c - 1) // EPOCH}", (c - 1) % EPOCH + 1, e))
    for s, v in S.dmacount.items():
        cur.append((s, v, None))
    for e in S.engs:
        waits = []
        for s, v, own in cur:
            if own == e:
                continue
            if S.seen[e].get(s, 0) >= v:
                continue
            S.seen[e][s] = v
            waits.append((s, v))
        if waits:
            S.ops[e].append((None, waits, None))


class Arena:
    def __init__(self, S, kb):
        self.S = S
        self.n = kb * 256
        self.t = S.tile([128, self.n], F32, "arena")
        self.k = 0

    def at(self, off_bytes, shape, dt, name=None, parts=128):
        self.k += 1
        esz = 2 if dt == BF16 else 4
        fsz = 1
        for s in shape[1:]:
            fsz *= s
        nby = fsz * esz
        assert off_bytes % 4 == 0
        assert off_bytes + nby <= self.n * 4, (name, off_bytes, nby)
        w = (nby + 3) // 4
        ap = self.t.ap[0:shape[0], off_bytes // 4: off_bytes // 4 + w]
        if dt != F32:
            ap = ap.bitcast(dt)
        if len(shape) == 3:
            ap = ap.rearrange("p (a b) -> p a b", a=shape[1])
        elif len(shape) == 4:
            ap = ap.rearrange("p (a b c) -> p a b c", a=shape[1], b=shape[2])
        return V(ap, Unit(name or f"ar{self.k}")), off_bytes + ((nby + 3) // 4) * 4


class Bump:
    def __init__(self, arena, start_kb, end_kb):
        self.a = arena
        self.off = start_kb * 1024
        self.end = end_kb * 1024

    def get(self, shape, dt, name=None):
        v, self.off = self.a.at(self.off, shape, dt, name)
        assert self.off <= self.end, (name, self.off, self.end)
        return v


def build_program(dbg=None, n_exp=NE, stages=("mix", "moe"), upto=None, nsb=NSB):
    nc = bass.Bass("TRN2", target_bir_lowering=False)

    def din(name, shape):
        return nc.dram_tensor(name, list(shape), F32, kind="ExternalInput").ap()

    x_d = din("x", [NT, D])
    g1_d = din("norm1_g", [1, D])
    g2_d = din("norm2_g", [1, D])
    gf_d = din("final_g", [1, D])
    win_d = din("w_in", [D, 2304])
    mu_d = din("mu_c", [128, 14])
    mu64_d = din("mu64_c", [64, 24])
    w0_d = din("w0_c", [64, 8])
    a0_d = din("a0_c", [64, 8])
    kk_d = din("kk_c", [64, 8])
    ka_d = din("ka_c", [64, 8])
    rk_d = din("rk_c", [64, 8])
    lnw_d = din("lnw", [1, 512])
    lnb_d = din("lnb", [1, 512])
    waup_d = din("wa_up", [128, 512])
    gup_d = din("g_up", [128, 512])
    lre_d = din("lam_re_c", [128, 16])
    lim_d = din("lam_im_c", [128, 16])
    ls_d = din("ls_c", [128, 16])
    xbre_d = din("xb_re", [128, 16, 128])
    xbim_d = din("xb_im", [128, 16, 128])
    ctre_d = din("ct_re", [128, 16, 64])
    ctim_d = din("ct_im", [128, 16, 64])
    dsk_d = din("dsk_c", [128, 4])
    bglu_d = din("bglu_c", [128, 4])
    beta_d = din("beta_c", [128, 4])
    wglu_d = din("w_glu", [512, 512])
    wout_d = din("w_out", [D, D])
    rw_d = din("router_w", [D, NE])
    rb_d = din("router_b", [1, NE])
    w1_d = din("w1r", [NE, 2, D, D])
    b1_d = din("b1_c", [128, NE, 2, 8])
    w2_d = din("w2", [NE, D, D])
    b2_d = din("b2", [NE, D])
    out_d = nc.dram_tensor("out", [NT, D], F32, kind="ExternalOutput").ap()
    dbg_d = None
    if dbg is not None:
        dbg_d = nc.dram_tensor("dbg", list(dbg[1]), F32, kind="ExternalOutput").ap()

    with contextlib.ExitStack() as st:
        S = Sched(nc, st)
        PS = [S.psum([128, 512], F32, "psb%d" % i) for i in range(8)]
        psi = [0]

        def nextps():
            p = PS[psi[0] % 8]
            psi[0] += 1
            return p

        ident_bf = S.tile([128, 128], BF16, "ident_bf")
        ident_f = S.tile([128, 128], F32, "ident_f")
        for t in (ident_bf, ident_f):
            S.memset(t, 0.0)
            S.affsel(t, t, [[-1, 128]], ALU.not_equal, 1.0, 0, 1)
        onesblk = S.tile([128, 128], F32, "onesblk")
        S.memset(onesblk, 0.0)
        S.memset(onesblk[0:64, 0:64], 1.0)
        S.memset(onesblk[64:128, 64:128], 1.0)
        ones_f = S.tile([128, 128], F32, "ones_f")
        S.memset(ones_f, 1.0)
        mhalf = S.tile([128, 8], F32, "mhalf")
        S.memset(mhalf, -0.5)
        hsel = S.tile([128, 2], BF16, "hsel")
        S.memset(hsel, 0.0)
        S.memset(hsel[0:64, 0:1], 1.0)
        S.memset(hsel[64:128, 1:2], 1.0)
        chunkmask = S.tile([128, SB], BF16, "chunkmask")
        S.memset(chunkmask, 1.0)
        S.memset(chunkmask.re("p (c l) -> p c l", l=LCH)[:, :, 0:1], 0.0)
        MUs = S.tile([64, 512], BF16, "MUs")
        MUi = S.tile([64, 512], BF16, "MUi")
        MLs = S.tile([64, 512], BF16, "MLs")
        I8 = S.tile([64, 512], BF16, "I8")
        for t in (MUs, MUi, MLs):
            S.memset(t, 1.0)
        S.memset(I8, 0.0)
        S.affsel(MUs, MUs, [[0, 8], [1, 64]], ALU.is_gt, 0.0, 0, -1)
        S.affsel(MUi, MUi, [[0, 8], [1, 64]], ALU.is_ge, 0.0, 0, -1)
        S.affsel(MLs, MLs, [[0, 8], [-1, 64]], ALU.is_gt, 0.0, 0, 1)
        S.affsel(I8, I8, [[0, 8], [-1, 64]], ALU.not_equal, 1.0, 0, 1)

        def ptile(d, shape, name, bc=None):
            t = S.tile(shape, F32, name)
            S.dma(t, d if bc is None else d.partition_broadcast(bc), "c0")
            return t

        mu = ptile(mu_d, [128, 14], "mu")
        mu64 = ptile(mu64_d, [64, 24], "mu64")
        w0c = ptile(w0_d, [64, 8], "w0c")
        a0c = ptile(a0_d, [64, 8], "a0c")
        kkc = ptile(kk_d, [64, 8], "kkc")
        kac = ptile(ka_d, [64, 8], "kac")
        rkc = ptile(rk_d, [64, 8], "rkc")
        dsk = ptile(dsk_d, [128, 4], "dsk")
        bglu = ptile(bglu_d, [128, 4], "bglu")
        betac = ptile(beta_d, [128, 4], "betac")
        lnw_b = S.tile([64, 512], BF16, "lnw_b")
        S.dma(lnw_b, lnw_d.partition_broadcast(64), "c1", eng="pool")
        lnb_b = S.tile([64, 512], BF16, "lnb_b")
        S.dma(lnb_b, lnb_d.partition_broadcast(64), "c1", eng="pool")
        rb_b = ptile(rb_d, [128, NE], "rb_b", bc=128)
        b1c = ptile(b1_d, [128, NE, 2, 8], "b1c")
        wa_up = S.tile([128, 512], BF16, "wa_up_sb")
        S.dma(wa_up, waup_d, "c1", eng="pool")
        g_up = S.tile([128, 512], BF16, "g_up_sb")
        S.dma(g_up, gup_d, "c1", eng="pool")
        rw_sb = S.tile([128, 8, NE], BF16, "rw_sb")
        S.dma(rw_sb, rw_d.rearrange("(k p) e -> p k e", p=128), "c1", eng="pool")
        ctre = S.tile([128, 16, 64], BF16, "ctre")
        S.dma(ctre, ctre_d, "c1", eng="pool")
        nctim = S.tile([128, 16, 64], BF16, "nctim")

        Sst = S.tile([64, 8, 64], F32, "Sst")
        Sbf = S.tile([64, 8, 64], BF16, "Sbf")
        S.memset(Sst, 0.0)
        S.memset(Sbf, 0.0)
        car_re = S.tile([128, 16], F32, "car_re")
        car_im = S.tile([128, 16], F32, "car_im")
        S.memset(car_re, 0.0)
        S.memset(car_im, 0.0)
        lastcol = S.tile([128, 14], F32, "lastcol")
        S.memset(lastcol, 0.0)
        lastcol64 = S.tile([64, 24], F32, "lastcol64")
        S.memset(lastcol64, 0.0)

        AR = Arena(S, ARENA_KB)

        bt = Bump(AR, 0, ARENA_KB)
        pw_re = S.tile([128, 12, 16], F32, "pw_re")
        pw_im = S.tile([128, 12, 16], F32, "pw_im")
        npw_im = S.tile([128, 12, 16], F32, "npw_im")
        BBT_re = S.tile([128, 16, 128], BF16, "BBT_re")
        BBT_im = S.tile([128, 16, 128], BF16, "BBT_im")
        if True:
            sm = [S.tile([128, 16], F32, "s5p%d" % i) for i in range(14)]
            lre, lim, lsc, dt_, t1, ang, kf, cosv, sinv, den, zre, zim, m1, m2 = sm
            ki = S.tile([128, 16], I32, "s5ki")
            for t, d in ((lre, lre_d), (lim, lim_d), (lsc, ls_d)):
                S.dma(t, d, "c0")
            xbre = bt.get([128, 16, 128], F32, "xbre")
            xbim = bt.get([128, 16, 128], F32, "xbim")
            bbx = bt.get([128, 128], F32, "bbx")
            bbx2 = bt.get([128, 128], F32, "bbx2")
            ctim_f = bt.get([128, 16, 64], F32, "ctim_f")
            S.dma(xbre, xbre_d, "c0")
            S.dma(xbim, xbim_d, "c0")
            S.dma(ctim_f, ctim_d, "c0")
            add_barrier(S)
            S.ts(b1c[:, :, 1, :], b1c[:, :, 1, :], 1.0, None, ALU.add)
            S.ts(w0c, w0c, -1.0, None, ALU.mult)
            S.ts(a0c, a0c, -1.0, None, ALU.mult)
            S.ts(lre, lre, -1e-4, None, ALU.min)
            S.act(dt_, lsc, AF.Exp)
            S.tt(t1, lre, dt_, ALU.mult)
            mag = pw_re[:, 0, :]
            S.act(t1, t1, AF.Exp)
            S.tt(ang, lim, dt_, ALU.mult)

            def sin_of(out, shift):
                S.ts(kf, ang, 1.0 / (2 * math.pi), shift / (2 * math.pi), ALU.mult, ALU.add)
                S.copy(ki, kf)
                S.copy(m1, ki)
                S.tt(kf, kf, m1, ALU.subtract)
                S.ts(m1, kf, 0.5, None, ALU.is_gt)
                S.ts(m2, kf, -0.5, None, ALU.is_lt)
                S.tt(kf, kf, m1, ALU.subtract)
                S.tt(kf, kf, m2, ALU.add)
                S.act(out, kf, AF.Sin, scale=2 * math.pi)

            sin_of(sinv, 0.0)
            sin_of(cosv, math.pi / 2)
            S.tt(pw_re[:, 0, :], t1, cosv, ALU.mult)
            S.tt(pw_im[:, 0, :], t1, sinv, ALU.mult)
            S.tt(den, lre, lre, ALU.mult)
            S.tt(m1, lim, lim, ALU.mult)
            S.tt(den, den, m1, ALU.add)
            S.recip(den, den)
            S.ts(m1, pw_re[:, 0, :], -1.0, None, ALU.add)
            S.tt(zre, m1, lre, ALU.mult)
            S.tt(m2, pw_im[:, 0, :], lim, ALU.mult)
            S.tt(zre, zre, m2, ALU.add)
            S.tt(zre, zre, den, ALU.mult)
            S.tt(zim, pw_im[:, 0, :], lre, ALU.mult)
            S.tt(m2, m1, lim, ALU.mult)
            S.tt(zim, zim, m2, ALU.subtract)
            S.tt(zim, zim, den, ALU.mult)
            for k in range(1, 12):
                S.tt(m1, pw_re[:, k - 1, :], pw_re[:, k - 1, :], ALU.mult)
                S.tt(m2, pw_im[:, k - 1, :], pw_im[:, k - 1, :], ALU.mult)
                S.tt(pw_re[:, k, :], m1, m2, ALU.subtract)
                S.tt(m1, pw_re[:, k - 1, :], pw_im[:, k - 1, :], ALU.mult)
                S.ts(pw_im[:, k, :], m1, 2.0, None, ALU.mult)
            S.ts(npw_im, pw_im, -1.0, None, ALU.mult)
            S.act(nctim, ctim_f, AF.Copy, scale=-1.0)
            for s in range(16):
                S.ts(bbx, xbim[:, s, :], zim[:, s:s + 1], -1.0, ALU.mult, ALU.mult)
                S.stt(bbx, xbre[:, s, :], zre[:, s:s + 1], bbx, ALU.mult, ALU.add)
                ps = nextps()
                S.tr(ps[:, 0:128], bbx, ident_f)
                S.copy(BBT_re[:, s, :], ps[:, 0:128], eng="act")
                S.ts(bbx2, xbre[:, s, :], zim[:, s:s + 1], None, ALU.mult)
                S.stt(bbx2, xbim[:, s, :], zre[:, s:s + 1], bbx2, ALU.mult, ALU.add)
                ps = nextps()
                S.tr(ps[:, 0:128], bbx2, ident_f)
                S.copy(BBT_im[:, s, :], ps[:, 0:128], eng="act")
        add_barrier(S)
        if upto == 'setup':
            S.emit()
            return nc

        for g in range(NG):
            gt0 = g * TG
            mixT, _ = AR.at(0, [128, 8, TG], BF16, "mixT")
            uT, _ = AR.at(32 * 1024, [128, 4, TG], BF16, "uT")
            if "mix" in stages:
                bt = Bump(AR, 48, ARENA_KB)
                w_in_sb = bt.get([128, 8, 2304], BF16, "w_in_sb")
                S.dma(w_in_sb, win_d.rearrange("(k p) c -> p k c", p=128), "win", eng="pool")
                g1b = bt.get([128, D], F32, "g1b")
                S.dma(g1b, g1_d.partition_broadcast(128), "g1b")
                xt = [bt.get([128, D], F32, "xt0")] * 2
                hb = [bt.get([128, D], BF16, "hb%d" % i) for i in range(2)]
                hT = bt.get([128, 8, SB], BF16, "hT")
                st4 = bt.get([128, 8], F32, "st4")
                raws = [bt.get([128, SB + 2], F32, "raw%d" % i) for i in range(2)]
                rawi = [0]
                p12 = bt.get([128, SB], F32, "p12")
                p13 = bt.get([128, SB], F32, "p13")
                prkv2 = [[bt.get([64, SB], F32, "prkv%d_%d" % (i, j)) for j in range(3)] for i in range(2)]
                tw = bt.get([128, SB], BF16, "tw")
                alr = bt.get([128, SB], BF16, "alr")
                sgl = bt.get([128, SB], BF16, "sgl")
                f32t2 = [[bt.get([64, SB], F32, "wk%d_%d" % (j, i)) for i in range(12)] for j in range(2)]
                f32t = f32t2[0]
                Gl = bt.get([64, NCH, 8], F32, "Gl")
                AT = bt.get([64, 8, SB], BF16, "AT")
                RT = bt.get([64, 8, SB], BF16, "RT")
                BT = bt.get([64, 8, SB], BF16, "BT")
                KT = bt.get([64, 8, SB], BF16, "KT")
                BhT = bt.get([64, 8, SB], BF16, "BhT")
                KhT = bt.get([64, 8, SB], BF16, "KhT")
                vT = bt.get([64, 8, SB], BF16, "vT")
                rkT = bt.get([64, 8, SB], BF16, "rkT")
                Atoks = [bt.get([64, 512], BF16, "Atok%d" % i) for i in range(NCH)]
                Bhat = [bt.get([64, 512], BF16, "Bhat%d" % i) for i in range(NCH)]
                Khat = [bt.get([64, 512], BF16, "Khat%d" % i) for i in range(NCH)]
                Vtok = [bt.get([64, 512], BF16, "Vtok%d" % i) for i in range(NCH)]
                Nbr = [bt.get([64, 512], BF16, "Nbr%d" % i) for i in range(NCH)]
                Nkr = [bt.get([64, 512], BF16, "Nkr%d" % i) for i in range(NCH)]
                W2 = [bt.get([64, 512], BF16, "W2_%d" % i) for i in range(NCH)]
                ApT = [bt.get([64, 512], BF16, "ApT%d" % i) for i in range(NCH)]
                Pms = [[bt.get([64, 512], BF16, "Pm%d_%d" % (i, j)) for j in range(2)] for i in range(NCH)]
                PTms = [[bt.get([64, 512], BF16, "PTm%d_%d" % (i, j)) for j in range(2)] for i in range(NCH)]
                Tms = [[bt.get([64, 512], BF16, "Tm%d_%d" % (i, j)) for j in range(2)] for i in range(NCH)]
                MakTs = [bt.get([64, 512], BF16, "MakT%d" % i) for i in range(NCH)]
                Usb = bt.get([64, 512], BF16, "Usb")
                ysbs = [bt.get([64, 512], F32, "ysb%d" % i) for i in range(NCH)]
                yo = bt.get([64, 512], BF16, "yo")
                vb = bt.get([64, 512], F32, "vb")
                sm8 = [bt.get([64, 8], F32, "sm8_%d" % i) for i in range(6)]

                def hc(h):
                    return slice(h * 64, (h + 1) * 64)

                def emit_norm(sb_):
                    tok0_ = gt0 + sb_ * SB
                    for t2 in range(SB // 128):
                        b = t2 % 2
                        S.dma(xt[b], x_d[tok0_ + t2 * 128: tok0_ + (t2 + 1) * 128, :], "x0")
                        S.act(hb[b], xt[b], AF.Square, accum=st4[:, 0:1])
                        S.ts(st4[:, 1:2], st4[:, 0:1], 1.0 / D, 1e-5, ALU.mult, ALU.add)
                        S.rsq(st4[:, 2:3], st4[:, 1:2])
                        S.stt(hb[b], xt[b], st4[:, 2:3], g1b, ALU.mult, ALU.mult)
                        pb = nextps().bitcast(BF16)
                        for k in range(8):
                            S.tr(pb[:, k * 128:(k + 1) * 128], hb[b][:, k * 128:(k + 1) * 128], ident_bf)
                        S.copy(hT[:, :, t2 * 128:(t2 + 1) * 128], pb.re("p (k t) -> p k t", k=8), eng="act")

                emit_norm(0)
                for sb in range(nsb):
                    tok0 = gt0 + sb * SB
                    lo = sb * SB
                    for t2 in range(0):
                        b = t2 % 2
                        S.dma(xt[b], x_d[tok0 + t2 * 128: tok0 + (t2 + 1) * 128, :], "x0")
                        S.act(hb[b], xt[b], AF.Square, accum=st4[:, 0:1])
                        S.ts(st4[:, 1:2], st4[:, 0:1], 1.0 / D, 1e-5, ALU.mult, ALU.add)
                        S.rsq(st4[:, 2:3], st4[:, 1:2])
                        S.stt(hb[b], xt[b], st4[:, 2:3], g1b, ALU.mult, ALU.mult)
                        pb = nextps().bitcast(BF16)
                        for k in range(8):
                            S.tr(pb[:, k * 128:(k + 1) * 128], hb[b][:, k * 128:(k + 1) * 128], ident_bf)
                        S.copy(hT[:, :, t2 * 128:(t2 + 1) * 128], pb.re("p (k t) -> p k t", k=8), eng="act")
                    if upto == 'R_h':
                        S.emit()
                        return nc

                    def proj(c0, w):
                        ps = nextps()
                        for k in range(8):
                            S.mm(ps[0:w, 0:SB], w_in_sb[:, k, c0:c0 + w], hT[:, k, :], start=(k == 0), stop=(k == 7))
                        return ps[0:w, 0:SB]

                    def shifted(c0, w, dst, lc, muc):
                        ps = proj(c0, w)
                        r_ = raws[rawi[0] % 2][0:w, :]
                        rawi[0] += 1
                        S.copy(r_[:, 0:1], lc)
                        S.copy(r_[:, 1:SB + 1], ps, eng="act")
                        S.copy(lc, r_[:, SB:SB + 1])
                        S.tt(dst, r_[:, 0:SB], r_[:, 1:SB + 1], ALU.subtract)
                        S.stt(dst, dst, muc, r_[:, 1:SB + 1], ALU.mult, ALU.add)

                    shifted(12 * 128, 128, p12, lastcol[:, 12:13], mu[:, 12:13])
                    twt = f32t2[1][11]
                    S.act(twt, p12[0:64, :], AF.Exp, scale=-2.0)
                    S.ts(twt, twt, 1.0, None, ALU.add)
                    S.recip(twt, twt)
                    S.ts(tw[0:64, :], twt, 2.0, -1.0, ALU.mult, ALU.add)
                    S.copy(alr[64:128, :], p12[64:128, :], eng="act")
                    shifted(13 * 128, 128, p13, lastcol[:, 13:14], mu[:, 13:14])
                    S.act(p13, p13, AF.Exp, scale=-1.0)
                    S.ts(p13, p13, 1.0, None, ALU.add)
                    S.recip(p13, p13)
                    S.copy(sgl, p13, eng="act")
                    c3 = lambda t: t.re("p (c l) -> p c l", l=LCH)
                    def S1(h):
                        pr, pk, pv = prkv2[h % 2]
                        sgw, cs, Gi, Ginv, cse, Ge, Ghat, a_, kk2, nrm, kap, kmod = f32t2[h % 2]
                        shifted(h * 64, 64, pr, lastcol64[:, h:h + 1], mu64[:, h:h + 1])
                        shifted(512 + h * 64, 64, pk, lastcol64[:, 8 + h:9 + h], mu64[:, 8 + h:9 + h])
                        shifted(1024 + h * 64, 64, pv, lastcol64[:, 16 + h:17 + h], mu64[:, 16 + h:17 + h])
                        psW = nextps()
                        S.mm(psW[0:64, 0:SB], wa_up[0:64, hc(h)], tw[0:64, :])
                        S.act(sgw, psW[0:64, 0:SB], AF.Exp, bias=w0c[:, h:h + 1], scale=-1.0)
                        psA = nextps()
                        S.mm(psA[0:64, 0:SB], wa_up[64:128, hc(h)], alr[64:128, :])
                        S.act(a_, psA[0:64, 0:SB], AF.Exp, bias=a0c[:, h:h + 1], scale=-1.0)
                        S.act(kk2, pk, AF.Square, scale=kkc[:, h:h + 1])
                        psN = nextps()
                        S.mm(psN[0:64, 0:SB], ones_f[0:64, 0:64], kk2)
                        S.ts(sgw, sgw, 1.0, None, ALU.add)
                        S.recip(sgw, sgw)
                        S.scan(cs, chunkmask[0:64, :], sgw, 0.0, ALU.mult, ALU.add)
                        S.ts(nrm, psN[0:64, 0:SB], 3e-19, None, ALU.max)
                        S.act(Gi, cs, AF.Exp, scale=-C0)
                        S.act(Ginv, cs, AF.Exp, scale=C0)
                        S.tt(cse, cs, sgw, ALU.subtract)
                        S.act(Ge, cse, AF.Exp, scale=-C0)
                        S.rsq(nrm, nrm)
                        S.ts(a_, a_, 1.0, None, ALU.add)
                        S.recip(a_, a_)

                    def S2(h):
                        pr, pk, pv = prkv2[h % 2]
                        sgw, cs, Gi, Ginv, cse, Ge, Ghat, a_, kk2, nrm, kap, kmod = f32t2[h % 2]
                        S.copy(Gl[:, :, h], c3(Gi)[:, :, LCH - 1])
                        S.tt(c3(Ghat), c3(Ginv), Gl[:, :, h:h + 1].bc([64, NCH, LCH]), ALU.mult)
                        S.stt(kap, pk, kkc[:, h:h + 1], nrm, ALU.mult, ALU.mult)
                        S.ts(kmod, a_, -1.0, kac[:, h:h + 1], ALU.add, ALU.mult)
                        S.stt(kmod, kmod, 1.0, pk, ALU.add, ALU.mult)
                        S.stt(AT[:, h, :], kap, -1.0, Ge, ALU.mult, ALU.mult)
                        S.tt(RT[:, h, :], pr, Gi, ALU.mult)
                        S.tt(kap, kap, a_, ALU.mult)
                        S.tt(BT[:, h, :], kap, Ginv, ALU.mult)
                        S.tt(KT[:, h, :], kmod, Ginv, ALU.mult)
                        S.tt(BhT[:, h, :], kap, Ghat, ALU.mult)
                        S.tt(KhT[:, h, :], kmod, Ghat, ALU.mult)
                        S.copy(vT[:, h, :], pv, eng="act")
                        S.stt(rkT[:, h, :], pr, rkc[:, h:h + 1], kmod, ALU.mult, ALU.mult)

                    S1(0)
                    for h in range(8):
                        if h + 1 < 8:
                            S1(h + 1)
                        S2(h)
                    for q in range(4):
                        ps = proj(1792 + q * 128, 128)
                        S.copy(uT[:, q, lo:lo + SB], ps, eng="act")
                    if upto == 'R_prep':
                        S.emit()
                        return nc

                    for c in range(NCH):
                        cs_ = slice(c * LCH, (c + 1) * LCH)
                        for src, dst in ((AT, Atoks[c]), (BhT, Bhat[c]), (KhT, Khat[c]), (vT, Vtok[c])):
                            pb = nextps().bitcast(BF16)
                            for h in range(8):
                                S.tr(pb[0:64, hc(h)], src[:, h, cs_], ident_bf[0:64, 0:64])
                            S.copy(dst, pb[0:64, 0:512], eng="act")

                        def prod(X, Y):
                            ps = nextps()
                            for h in range(8):
                                S.mm(ps[0:64, hc(h)], X[:, h, cs_], Y[:, h, cs_])
                            return ps[0:64, :]

                        P, PT, T = Pms[c][0], PTms[c][0], Tms[c][0]
                        ps = prod(BT, AT)
                        S.tt(P, ps, MUs, ALU.mult)
                        S.tt(T, P, I8, ALU.add)
                        ps = prod(BT, RT)
                        S.tt(Nbr[c], ps, MUi, ALU.mult)
                        ps = prod(KT, RT)
                        S.tt(Nkr[c], ps, MUi, ALU.mult)
                        ps = prod(AT, KT)
                        S.tt(MakTs[c], ps, MLs, ALU.mult)
                        ps = prod(AT, BT)
                        S.tt(PT, ps, MLs, ALU.mult)
                    cur = 0
                    for lvl in range(1, 6):
                        for c in range(NCH):
                            P, PT, T = Pms[c][cur], PTms[c][cur], Tms[c][cur]
                            P2, PT2, T2 = Pms[c][1 - cur], PTms[c][1 - cur], Tms[c][1 - cur]
                            if lvl < 5:
                                psP = nextps()
                                for h in range(8):
                                    S.mm(psP[0:64, hc(h)], PT[:, hc(h)], P[:, hc(h)])
                                S.copy(P2, psP[0:64, :], eng="act")
                            psPT = nextps()
                            for h in range(8):
                                S.mm(psPT[0:64, hc(h)], P[:, hc(h)], PT[:, hc(h)])
                            S.copy(PT2, psPT[0:64, :], eng="dve")
                        for c in range(NCH):
                            T, PT2, T2 = Tms[c][cur], PTms[c][1 - cur], Tms[c][1 - cur]
                            psT = nextps()
                            for h in range(8):
                                S.mm(psT[0:64, hc(h)], PT2[:, hc(h)], T[:, hc(h)])
                            S.tt(T2, psT[0:64, :], T, ALU.add)
                        cur = 1 - cur
                    for c in range(NCH):
                        T = Tms[c][cur]
                        psA = nextps()
                        for h in range(8):
                            S.mm(psA[0:64, hc(h)], Atoks[c][:, hc(h)], T[:, hc(h)])
                        S.copy(ApT[c], psA[0:64, :], eng="act")
                        psW = nextps()
                        for h in range(8):
                            S.mm(psW[0:64, hc(h)], MakTs[c][:, hc(h)], T[:, hc(h)])
                        S.copy(W2[c], psW[0:64, :], eng="dve")
                    if upto == 'R_chunk':
                        S.emit()
                        return nc

                    if sb + 1 < nsb:
                        emit_norm(sb + 1)
                    for c in range(NCH):
                        cs_ = slice(c * LCH, (c + 1) * LCH)
                        psU = nextps()
                        for h in range(8):
                            S.mm(psU[0:64, hc(h)], ApT[c][:, hc(h)], Sbf[:, h, :], start=True, stop=False)
                            S.mm(psU[0:64, hc(h)], W2[c][:, hc(h)], Vtok[c][:, hc(h)], start=False, stop=True)
                        S.copy(Usb, psU[0:64, :], eng="act")
                        psY = nextps()
                        for h in range(8):
                            S.mm(psY[0:64, hc(h)], RT[:, h, cs_], Sbf[:, h, :], start=True, stop=False)
                            S.mm(psY[0:64, hc(h)], Nbr[c][:, hc(h)], Usb[:, hc(h)], start=False, stop=False)
                            S.mm(psY[0:64, hc(h)], Nkr[c][:, hc(h)], Vtok[c][:, hc(h)], start=False, stop=True)
                        psS = nextps()
                        for h in range(8):
                            S.mm(psS[0:64, hc(h)], Bhat[c][:, hc(h)], Usb[:, hc(h)], start=True, stop=False)
                            S.mm(psS[0:64, hc(h)], Khat[c][:, hc(h)], Vtok[c][:, hc(h)], start=False, stop=True)
                        S.tt(Sst, Sst, Gl[:, c, :].re("p (a o) -> p a o", o=1).bc([64, 8, 64]), ALU.mult)
                        S.tt(Sst, Sst, psS[0:64, :].re("p (a v) -> p a v", a=8), ALU.add)
                        S.copy(Sbf, Sst, eng="act")
                        S.copy(ysbs[c], psY[0:64, :], eng="act")
                    for c in range(NCH):
                        cs_ = slice(c * LCH, (c + 1) * LCH)
                        ysb = ysbs[c]
                        s1, s2, mn, var, rs, bon = sm8
                        y3 = lambda t: t.re("p (h n) -> p h n", h=8)
                        b8 = lambda t: t.re("p (h o) -> p h o", o=1).bc([64, 8, 64])
                        if upto == 'R' and g == 0 and sb == 0 and c == 0:
                            S.dma(dbg_d[0:64, 7, 0:512], ysb, "dbg2")
                        for h in range(8):
                            S.act(vb[:, hc(h)], ysb[:, hc(h)], AF.Identity, accum=s1[:, h:h + 1])
                            S.act(vb[:, hc(h)], ysb[:, hc(h)], AF.Square, accum=s2[:, h:h + 1])
                        S.ts(mn, s1, 1.0 / 64, None, ALU.mult)
                        S.tt(var, mn, mn, ALU.mult)
                        S.stt(var, s2, 1.0 / 64, var, ALU.mult, ALU.subtract)
                        S.ts(var, var, 64e-5, None, ALU.add)
                        S.rsq(rs, var)
                        S.tt(mn, mn, rs, ALU.mult)
                        S.ts(mn, mn, -1.0, None, ALU.mult)
                        for h in range(8):
                            S.act(ysb[:, hc(h)], ysb[:, hc(h)], AF.Identity, bias=mn[:, h:h + 1], scale=rs[:, h:h + 1])
                        if upto == 'R' and g == 0 and sb == 0 and c == 0:
                            for i_, t_ in enumerate(sm8[:5]):
                                S.dma(dbg_d[0:64, 7, 512 + 8 * i_: 520 + 8 * i_], t_, "dbg2")
                            S.dma(dbg_d[0:64, 7, 1024:1536], ysb, "dbg2")
                        S.tt(ysb, ysb, lnw_b, ALU.mult)
                        S.tt(ysb, ysb, lnb_b, ALU.add)
                        psB = nextps()
                        for h in range(8):
                            S.mm(psB[0:64, h:h + 1], rkT[:, h, cs_], hsel[0:64, 0:1])
                        S.copy(bon, psB[0:64, 0:8], eng="act")
                        S.tt(y3(vb), y3(Vtok[c]), b8(bon), ALU.mult)
                        S.tt(ysb, ysb, vb, ALU.add)
                        psG = nextps()
                        S.mm(psG[0:64, :], sgl[:, cs_], g_up)
                        S.tt(yo, ysb, psG[0:64, :], ALU.mult)
                        pb = nextps().bitcast(BF16)
                        for hp in range(4):
                            S.tr(pb[:, hp * 64:(hp + 1) * 64], yo[:, hp * 128:(hp + 1) * 128], ident_bf[0:64, 0:64])
                        S.copy(mixT[:, 0:4, lo + c * LCH: lo + (c + 1) * LCH], pb[:, 0:256].re("p (a t) -> p a t", a=4), eng="act")
                add_barrier(S)
                if upto == 'R':
                    dsb, _ = AR.at(48 * 1024, [128, 8, TG], F32, 'dbgsb')
                    S.copy(dsb, mixT)
                    S.dma(dbg_d[:, 0:7, :], dsb[:, 0:7, :], 'dbg')
                    S.emit()
                    return nc

                bt = Bump(AR, 48, ARENA_KB)
                A = [[bt.get([128, TG], F32, "s5A%d%d" % (i, j)) for j in range(2)] for i in range(2)]
                sbf = [bt.get([128, TG], BF16, "s5sbf%d" % i) for i in range(2)]
                yTq = bt.get([128, TG], F32, "yTq")
                geT = bt.get([128, 4, TG], BF16, "geT")
                glo = bt.get([128, 4, TG], F32, "glo")
                wk = [bt.get([128, 512], F32, "s5wk%d" % i) for i in range(3)]
                wglu = bt.get([128, 4, 512], BF16, "wglu_sb")
                S.dma(wglu, wglu_d.rearrange("(k p) c -> p k c", p=128), "wglu", eng="pool")
                nblk = TG // 512
                for s in range(16):
                    q, r4 = s // 4, s % 4
                    for blk in range(nblk):
                        bs = slice(blk * 512, (blk + 1) * 512)
                        ps = nextps()
                        S.mm(ps, BBT_re[:, s, :], uT[:, q, bs])
                        S.copy(A[0][0][:, bs], ps, eng="act")
                        ps = nextps()
                        S.mm(ps, BBT_im[:, s, :], uT[:, q, bs])
                        S.copy(A[0][1][:, bs], ps, eng="act")
                    S.stt(A[0][0][:, 0:1], car_re[:, s:s + 1], pw_re[:, 0, s:s + 1], A[0][0][:, 0:1], ALU.mult, ALU.add)
                    S.stt(A[0][0][:, 0:1], car_im[:, s:s + 1], npw_im[:, 0, s:s + 1], A[0][0][:, 0:1], ALU.mult, ALU.add)
                    S.stt(A[0][1][:, 0:1], car_im[:, s:s + 1], pw_re[:, 0, s:s + 1], A[0][1][:, 0:1], ALU.mult, ALU.add)
                    S.stt(A[0][1][:, 0:1], car_re[:, s:s + 1], pw_im[:, 0, s:s + 1], A[0][1][:, 0:1], ALU.mult, ALU.add)
                    cur = 0
                    k = 0
                    d = 1
                    while d < TG:
                        a, b = A[cur], A[1 - cur]
                        ar, ai, nai = pw_re[:, k, s:s + 1], pw_im[:, k, s:s + 1], npw_im[:, k, s:s + 1]
                        S.stt(b[0][:, d:], a[0][:, 0:TG - d], ar, a[0][:, d:], ALU.mult, ALU.add)
                        S.stt(b[0][:, d:], a[1][:, 0:TG - d], nai, b[0][:, d:], ALU.mult, ALU.add)
                        S.stt(b[1][:, d:], a[1][:, 0:TG - d], ar, a[1][:, d:], ALU.mult, ALU.add)
                        S.stt(b[1][:, d:], a[0][:, 0:TG - d], ai, b[1][:, d:], ALU.mult, ALU.add)
                        S.copy(b[0][:, 0:d], a[0][:, 0:d], eng="act")
                        S.copy(b[1][:, 0:d], a[1][:, 0:d], eng="act")
                        cur = 1 - cur
                        d *= 2
                        k += 1
                    fin = A[cur]
                    S.copy(car_re[:, s:s + 1], fin[0][:, TG - 1:TG])
                    S.copy(car_im[:, s:s + 1], fin[1][:, TG - 1:TG])
                    S.copy(sbf[0], fin[0], eng="act")
                    S.copy(sbf[1], fin[1], eng="act")
                    for blk in range(nblk):
                        bs = slice(blk * 512, (blk + 1) * 512)
                        ps = nextps()
                        pb0 = (r4 // 2) * 64
                        o = ps[pb0:pb0 + 64, :]
                        S.mm(o, ctre[:, s, :], sbf[0][:, bs], start=True, stop=False)
                        S.mm(o, nctim[:, s, :], sbf[1][:, bs], start=False, stop=True)
                        if r4 % 2 == 0:
                            S.copy(yTq[pb0:pb0 + 64, bs], o, eng="act")
                        else:
                            S.tt(yTq[pb0:pb0 + 64, bs], yTq[pb0:pb0 + 64, bs], o, ALU.add)
                    if r4 == 3:
                        for blk in range(nblk):
                            bs = slice(blk * 512, (blk + 1) * 512)
                            yv, x2_, t_ = wk
                            S.stt(yv, uT[:, q, bs], dsk[:, q:q + 1], yTq[:, bs], ALU.mult, ALU.add)
                            S.act(x2_, yv, AF.Square)
                            S.ts(x2_, x2_, 0.044715, 1.0, ALU.mult, ALU.add)
                            S.tt(x2_, x2_, yv, ALU.mult)
                            S.act(t_, x2_, AF.Sigmoid, scale=2.0 * math.sqrt(2.0 / math.pi))
                            S.tt(geT[:, q, bs], yv, t_, ALU.mult)
                for blk in range(nblk):
                    bs = slice(blk * 512, (blk + 1) * 512)
                    psn = nextps()
                    for q in range(4):
                        ps = nextps()
                        for k in range(4):
                            S.mm(ps, wglu[:, k, q * 128:(q + 1) * 128], geT[:, k, bs], start=(k == 0), stop=(k == 3))
                        gl_, sq_, _ = wk
                        S.act(gl_, ps, AF.Sigmoid, bias=bglu[:, q:q + 1])
                        S.tt(glo[:, q, bs], geT[:, q, bs], gl_, ALU.mult)
                        S.act(sq_, glo[:, q, bs], AF.Square)
                        S.mm(psn, ones_f, sq_, start=(q == 0), stop=(q == 3))
                    rst = wk[2]
                    S.ts(rst, psn, 1.0 / 512, 1e-5, ALU.mult, ALU.add)
                    S.act(rst, rst, AF.Sqrt)
                    S.recip(rst, rst)
                    for q in range(4):
                        S.stt(mixT[:, 4 + q, bs], glo[:, q, bs], betac[:, q:q + 1], rst, ALU.mult, ALU.mult)
                add_barrier(S)

            if dbg is not None and dbg[0] == "mixT" and g == 0:
                dsb, _ = AR.at(48 * 1024, [128, 8, TG], F32, "dbgsb")
                S.copy(dsb, mixT)
                S.dma(dbg_d, dsb, "dbg")
                add_barrier(S)

            X2, _ = AR.at(48 * 1024, [128, 16, D], F32, "X2")
            h2T, _ = AR.at(112 * 1024, [128, 8, TG], BF16, "h2T")
            bt = Bump(AR, 144, ARENA_KB)
            wout = bt.get([128, 8, D], BF16, "wout_sb")
            S.dma(wout, wout_d.rearrange("(k p) c -> p k c", p=128), "win", eng="pool")
            g2b = bt.get([128, D], F32, "g2b")
            S.dma(g2b, g2_d.partition_broadcast(128), "c0")
            xt = [bt.get([128, D], F32, "xt2_0")] * 2
            hb = [bt.get([128, D], BF16, "hb2_%d" % i) for i in range(2)]
            b2_sb = bt.get([NE, D], F32, "b2_sb")
            S.dma(b2_sb, b2_d, "b2")
            COMB, _ = AR.at(32 * 1024, [128, 16, NE], F32, "COMB")
            COMBS, _ = AR.at(34 * 1024, [128, 16, NE], F32, "COMBS")
            combT, _ = AR.at(36 * 1024, [NE, TG], F32, "combT")
            rt = [AR.at((44 * 1024) + i * 128, [128, NE], F32, "rt%d" % i)[0] for i in range(3)]
            rs8 = AR.at(44 * 1024 + 512, [128, 16], F32, "rs8")[0]
            for t2 in range(16):
                b = t2 % 2
                ts_ = slice(t2 * 128, (t2 + 1) * 128)
                S.dma(xt[b], x_d[gt0 + t2 * 128: gt0 + (t2 + 1) * 128, :], "x0")
                for half in range(2):
                    ps = nextps()
                    for k in range(8):
                        S.mm(ps, mixT[:, k, ts_], wout[:, k, half * 512:(half + 1) * 512], start=(k == 0), stop=(k == 7))
                    S.tt(X2[:, t2, half * 512:(half + 1) * 512], xt[b][:, half * 512:(half + 1) * 512], ps, ALU.add)
                S.act(hb[b], X2[:, t2, :], AF.Square, accum=rs8[:, 0:1])
                S.ts(rs8[:, 1:2], rs8[:, 0:1], 1.0 / D, 1e-5, ALU.mult, ALU.add)
                S.rsq(rs8[:, 2:3], rs8[:, 1:2])
                S.stt(hb[b], X2[:, t2, :], rs8[:, 2:3], g2b, ALU.mult, ALU.mult)
                pb = nextps().bitcast(BF16)
                for k in range(8):
                    S.tr(pb[:, k * 128:(k + 1) * 128], hb[b][:, k * 128:(k + 1) * 128], ident_bf)
                S.copy(h2T[:, :, ts_], pb.re("p (k t) -> p k t", k=8), eng="act")
                ps = nextps()
                for k in range(8):
                    S.mm(ps[:, 0:NE], h2T[:, k, ts_], rw_sb[:, k, :], start=(k == 0), stop=(k == 7))
                lg, ex, mk = rt
                S.tt(lg, ps[:, 0:NE], rb_b, ALU.add)
                S.op("dve", lambda e, o=rs8[:, 8:16], i=lg: e.max(o.ap, i.ap), reads=[lg], writes=[rs8])
                S.ts(mk, lg, rs8[:, 11:12], None, ALU.is_ge)
                S.ts(rs8[:, 3:4], rs8[:, 8:9], -1.0, None, ALU.mult)
                S.act(ex, lg, AF.Exp, bias=rs8[:, 3:4])
                S.tt(ex, ex, mk, ALU.mult)
                S.reduce(rs8[:, 4:5], ex, ALU.add)
                S.recip(rs8[:, 5:6], rs8[:, 4:5])
                S.ts(COMB[:, t2, :], ex, rs8[:, 5:6], None, ALU.mult)
                S.ts(COMBS[:, t2, :], COMB[:, t2, :], 1.0 / 1.702, None, ALU.mult)
                ps = nextps()
                S.tr(ps[0:NE, 0:128], COMB[:, t2, :], ident_f)
                S.copy(combT[:, ts_], ps[0:NE, 0:128], eng="act")
                for half in range(2):
                    ps = nextps()
                    S.mm(ps, combT[:, ts_], b2_sb[:, half * 512:(half + 1) * 512])
                    S.tt(X2[:, t2, half * 512:(half + 1) * 512], X2[:, t2, half * 512:(half + 1) * 512], ps, ALU.add)
            add_barrier(S)

            if dbg is not None and dbg[0] == "X2" and g == 0:
                S.dma(dbg_d, X2, "dbg")
                add_barrier(S)

            if "moe" in stages:
                actT, _ = AR.at(0, [128, 8, TG], BF16, "actT")
                w2b = [AR.at((144 + 16 * i) * 1024, [128, 8, D], BF16, "w2b%d" % i)[0] for i in range(2)]
                w1b = [AR.at(36 * 1024 + i * 4096, [128, 2, 8, 128], BF16, "w1b%d" % i)[0] for i in range(2)]
                ew = [AR.at(32 * 1024 + i * 1024, [128, 512], BF16, "ew%d" % i)[0] for i in range(2)]
                exg, _ = AR.at(46 * 1024, [128, 512], F32, "exg")
                exl, _ = AR.at(44 * 1024, [128, 512], F32, "exl")
                nf = 0
                for e in range(n_exp):
                    wb = w2b[e % 2]
                    S.dma(wb, w2_d[e].rearrange("(k p) c -> p k c", p=128), "w2_%d" % (e % 2), eng="pool")
                    for f in range(8):
                        w1 = w1b[nf % 2]
                        S.dma(w1, w1_d[e, :, :, f * 128:(f + 1) * 128].rearrange("g (k p) m -> p g k m", p=128),
                              "w1_%d" % (nf % 2), eng="pool")
                        nf += 1
                        for tb in range(TG // 512):
                            bs = slice(tb * 512, (tb + 1) * 512)
                            pg = nextps()
                            for k in range(8):
                                S.mm(pg, w1[:, 0, k, :], h2T[:, k, bs], start=(k == 0), stop=(k == 7))
                            pl = nextps()
                            for k in range(8):
                                S.mm(pl, w1[:, 1, k, :], h2T[:, k, bs], start=(k == 0), stop=(k == 7))
                            S.ts(exg, pg, b1c[:, e, 0, f:f + 1], 7.0, ALU.add, ALU.min)
                            S.act(ew[0], exg, AF.Silu, scale=1.702)
                            S.act(exl, pl, AF.Identity, bias=b1c[:, e, 1, f:f + 1])
                            S.ts(ew[1], exl, -6.0, 8.0, ALU.max, ALU.min)
                            S.tt(actT[:, f, bs], ew[0], ew[1], ALU.mult)
                    for t2 in range(16):
                        ts_ = slice(t2 * 128, (t2 + 1) * 128)
                        for half in range(2):
                            ps = nextps()
                            for f in range(8):
                                S.mm(ps, actT[:, f, ts_], wb[:, f, half * 512:(half + 1) * 512], start=(f == 0), stop=(f == 7))
                            xs = X2[:, t2, half * 512:(half + 1) * 512]
                            S.stt(xs, ps, COMBS[:, t2, e:e + 1], xs, ALU.mult, ALU.add)
                add_barrier(S)

            gfb, _ = AR.at(0, [128, D], F32, "gfb")
            ob = [AR.at(4096 * (1 + i), [128, D], F32, "ob%d" % i)[0] for i in range(2)]
            rs8 = AR.at(4096 * 4, [128, 16], F32, "rs8f")[0]
            S.dma(gfb, gf_d.partition_broadcast(128), "c0")
            for t2 in range(16):
                b = t2 % 2
                S.act(ob[b], X2[:, t2, :], AF.Square, accum=rs8[:, 0:1])
                S.ts(rs8[:, 1:2], rs8[:, 0:1], 1.0 / D, 1e-5, ALU.mult, ALU.add)
                S.rsq(rs8[:, 2:3], rs8[:, 1:2])
                S.stt(ob[b], X2[:, t2, :], rs8[:, 2:3], gfb, ALU.mult, ALU.mult)
                S.dma(out_d[gt0 + t2 * 128: gt0 + (t2 + 1) * 128, :], ob[b], "out%d" % b)
            add_barrier(S)
        S.emit()
    return nc


def _prep_inputs(inp):
    f = lambda a: np.ascontiguousarray(np.asarray(a, dtype=np.float32))
    col = lambda v, m: f(np.asarray(v).reshape(m, 128).T)
    d = {}
    d["norm1_g"] = f(inp["norm1_g"][0][None])
    d["norm2_g"] = f(inp["norm2_g"][0][None])
    d["final_g"] = f(np.asarray(inp["final_g"])[None])
    d["w_in"] = f(inp["w_in"][0])
    d["mu_c"] = col(inp["mu_shift"][0], 14)
    col64 = lambda v, m: f(np.asarray(v).reshape(m, 64).T)
    d["mu64_c"] = col64(np.asarray(inp["mu_shift"][0])[:1536], 24)
    d["w0_c"] = col64(inp["w0"][0], 8)
    d["a0_c"] = col64(inp["a0"][0], 8)
    d["kk_c"] = col64(inp["k_k"][0], 8)
    d["ka_c"] = col64(inp["k_a"][0], 8)
    d["rk_c"] = col64(np.asarray(inp["r_k"][0]).reshape(-1), 8)
    d["lnw"] = f(inp["ln_x_w"][0][None])
    d["lnb"] = f(inp["ln_x_b"][0][None])
    d["wa_up"] = f(np.concatenate([inp["w_up"][0], inp["a_up"][0]], 0))
    d["g_up"] = f(inp["g_up"][0])
    d["lam_re_c"] = col(inp["lambda_re"][0].reshape(-1), 16)
    d["lam_im_c"] = col(inp["lambda_im"][0].reshape(-1), 16)
    d["ls_c"] = col(np.repeat(np.asarray(inp["log_step"][0]), 64), 16)
    for nm, src in (("xb_re", inp["b_re"][0]), ("xb_im", inp["b_im"][0])):
        src = np.asarray(src)
        xb = np.zeros((128, 16, 128), np.float32)
        for s in range(16):
            for gl in range(2):
                c0 = 32 * (s % 4) + gl * 16
                xb[gl * 64:(gl + 1) * 64, s, c0:c0 + 16] = src[2 * s + gl]
        d[nm] = xb
    for nm, src in (("ct_re", inp["c_re"][0]), ("ct_im", inp["c_im"][0])):
        src = np.asarray(src)
        ct = np.zeros((128, 16, 64), np.float32)
        for s in range(16):
            for gl in range(2):
                c0 = (s % 2) * 32 + gl * 16
                ct[gl * 64:(gl + 1) * 64, s, c0:c0 + 16] = src[2 * s + gl].T
        d[nm] = ct
    d["dsk_c"] = col(inp["d_skip"][0], 4)
    d["bglu_c"] = col(inp["b_glu"][0], 4)
    d["beta_c"] = col(inp["beta_ssm"][0], 4)
    d["w_glu"] = f(inp["w_glu"][0])
    d["w_out"] = f(inp["w_out"][0])
    d["router_w"] = f(inp["router_w"][0])
    d["router_b"] = f(inp["router_b"][0][None])
    w1 = np.asarray(inp["w1"][0])
    d["w1r"] = f(w1.reshape(NE, D, D, 2).transpose(0, 3, 1, 2))
    d["b1_c"] = f(np.asarray(inp["b1"][0]).reshape(NE, 8, 128, 2).transpose(2, 0, 3, 1))
    d["w2"] = f(inp["w2"][0])
    d["b2"] = f(inp["b2"][0])
    return d


def kernel(**inputs):
    shared = _prep_inputs(inputs)
    x = np.asarray(inputs["x"], dtype=np.float32)
    nc = build_program()
    in_maps = []
    for c in range(8):
        m = dict(shared)
        m["x"] = np.ascontiguousarray(x[c])
        in_maps.append(m)
    res = run_bass_kernel_spmd(nc, in_maps, core_ids=list(range(8)))
    return np.stack([np.asarray(r["out"]) for r in res.results], 0).astype(np.float32)
```
